# Optimizing a Trainium2 kernel written in Bass

```python
import jax
import jax.numpy as jnp
from jax import lax
import numpy as np

D_MODEL = 2048
BATCH = 4
SEQ = 4096
DEPTH = 1

N_Q_HEADS = 32
N_KV_HEADS = 4
HEAD_DIM = 64
Q_WIDTH = N_Q_HEADS * HEAD_DIM
KV_WIDTH = N_KV_HEADS * HEAD_DIM
WINDOW = 128
LRU_WIDTH = D_MODEL
LRU_BLOCKS = 8
LRU_BLOCK_WIDTH = LRU_WIDTH // LRU_BLOCKS
CONV_WIDTH = 4
LRU_C = 8.0
IN_SPLITS = (Q_WIDTH, KV_WIDTH, KV_WIDTH, LRU_WIDTH, LRU_WIDTH, D_MODEL, D_MODEL)
IN_WIDTH = sum(IN_SPLITS)
IN_CUTS = [sum(IN_SPLITS[:i + 1]) for i in range(len(IN_SPLITS) - 1)]
N_EXPERTS = 32
TOP_K = 4
D_EXPERT = D_MODEL
SWIGLU_LIMIT = 7.0
SWIGLU_ALPHA = 1.702
EXPERT_BLOCK = 256
PLE_DIM = 256
RMS_EPS = 1e-6

kernel_name = 'hybrid_swa_sink_rglru_moe_block'


def rmsnorm(x, g):
    xf = x.astype(jnp.float32)
    var = jnp.mean(xf * xf, axis=-1, keepdims=True)
    return (xf * lax.rsqrt(var + RMS_EPS)).astype(x.dtype) * g


def sliding_window_attention(q, k, v, sinks):
    b, s, _ = q.shape
    nb = s // WINDOW
    g = N_Q_HEADS // N_KV_HEADS
    qb = q.reshape(b, nb, WINDOW, N_KV_HEADS, g, HEAD_DIM)

    def band(t):
        tb = t.reshape(b, nb, WINDOW, N_KV_HEADS, HEAD_DIM)
        prev = jnp.pad(tb[:, :-1], ((0, 0), (1, 0), (0, 0), (0, 0), (0, 0)))
        return jnp.concatenate([prev, tb], axis=2)

    kk, vv = band(k), band(v)
    scores = jnp.einsum('bnqkgd,bnjkd->bnkgqj', qb, kk).astype(jnp.float32) * (HEAD_DIM ** -0.5)
    i = jnp.arange(WINDOW)[:, None]
    j = jnp.arange(2 * WINDOW)[None, :]
    in_band = (j > i) & (j <= i + WINDOW)
    has_prev = (jnp.arange(nb) > 0)[:, None, None] | (j >= WINDOW)[None]
    mask = in_band[None] & has_prev
    scores = jnp.where(mask[None, :, None, None], scores, jnp.finfo(jnp.float32).min)
    sink = jnp.broadcast_to(sinks.astype(jnp.float32).reshape(1, 1, N_KV_HEADS, g, 1, 1),
                            scores.shape[:-1] + (1,))
    probs = jax.nn.softmax(jnp.concatenate([scores, sink], axis=-1), axis=-1)[..., :-1]
    out = jnp.einsum('bnkgqj,bnjkd->bnqkgd', probs.astype(v.dtype), vv)
    return out.reshape(b, s, Q_WIDTH)


def rglru_branch(u, gate, conv_w, conv_b, w_rg_a, b_rg_a, w_rg_x, b_rg_x, lam):
    b, s, w = u.shape
    upad = jnp.pad(u, ((0, 0), (CONV_WIDTH - 1, 0), (0, 0)))
    uc = conv_b + upad[:, 0:s] * conv_w[0]
    for tap in range(1, CONV_WIDTH):
        uc = uc + upad[:, tap:tap + s] * conv_w[tap]
    ub = uc.reshape(b, s, LRU_BLOCKS, LRU_BLOCK_WIDTH)
    r = jax.nn.sigmoid(jnp.einsum('bsnc,ncd->bsnd', ub, w_rg_a) + b_rg_a).reshape(b, s, w)
    ig = jax.nn.sigmoid(jnp.einsum('bsnc,ncd->bsnd', ub, w_rg_x) + b_rg_x).reshape(b, s, w)
    log_a = (-LRU_C * r.astype(jnp.float32)) * jax.nn.softplus(-lam.astype(jnp.float32))
    a = jnp.exp(log_a)
    mult = jnp.sqrt(-jnp.expm1(2.0 * log_a))
    mult = jnp.where((jnp.arange(s) == 0)[None, :, None], 1.0, mult)
    xin = mult * (ig * uc).astype(jnp.float32)

    def combine(left, right):
        a_l, b_l = left
        a_r, b_r = right
        return a_l * a_r, a_r * b_l + b_r

    _, h = lax.associative_scan(combine, (a, xin), axis=1)
    return h.astype(u.dtype) * jax.nn.gelu(gate)


def moe(xn, w_router, b_router, w1, b1, w2, b2):
    b, s, d = xn.shape
    n = b * s
    xt = xn.reshape(n, d)
    logits = (xt @ w_router + b_router).astype(jnp.float32)
    top_val, top_idx = lax.top_k(logits, TOP_K)
    top_w = jax.nn.softmax(top_val, axis=-1).astype(xn.dtype)
    nk = n * TOP_K
    e_flat = top_idx.reshape(nk).astype(jnp.int32)
    tok_flat = jnp.arange(nk, dtype=jnp.int32) // TOP_K
    w_flat = top_w.reshape(nk)
    order = jnp.argsort(e_flat)
    e_sorted = e_flat[order]
    counts = jnp.bincount(e_flat, length=N_EXPERTS).astype(jnp.int32)
    padded = (counts + EXPERT_BLOCK - 1) // EXPERT_BLOCK * EXPERT_BLOCK
    start = jnp.cumsum(counts) - counts
    pend = jnp.cumsum(padded)
    pstart = pend - padded
    dest = pstart[e_sorted] + jnp.arange(nk, dtype=jnp.int32) - start[e_sorted]
    n_rows = (-(-nk // EXPERT_BLOCK) + N_EXPERTS) * EXPERT_BLOCK
    row_tok = jnp.full((n_rows,), n, jnp.int32).at[dest].set(tok_flat[order])
    row_w = jnp.zeros((n_rows,), xn.dtype).at[dest].set(w_flat[order])
    n_blk = n_rows // EXPERT_BLOCK
    blk_start = jnp.arange(n_blk, dtype=jnp.int32) * EXPERT_BLOCK
    blk_expert = jnp.minimum(jnp.searchsorted(pend, blk_start, side='right'), N_EXPERTS - 1)
    x_rows = jnp.concatenate([xt, jnp.zeros((1, d), xt.dtype)], axis=0)[row_tok]
    x_rows = x_rows.reshape(n_blk, EXPERT_BLOCK, d)

    def expert_block(args):
        xb, e = args
        hmid = xb @ w1[e] + b1[e]
        glu, lin = jnp.split(hmid, 2, axis=-1)
        glu = jnp.minimum(glu, SWIGLU_LIMIT)
        lin = jnp.clip(lin, -SWIGLU_LIMIT, SWIGLU_LIMIT)
        act = glu * jax.nn.sigmoid(SWIGLU_ALPHA * glu) * (lin + 1.0)
        return act @ w2[e] + b2[e]

    y_rows = lax.map(expert_block, (x_rows, blk_expert)).reshape(n_rows, d)
    y = jax.ops.segment_sum(y_rows * row_w[:, None], row_tok, num_segments=n + 1)[:n]
    return y.reshape(b, s, d)


def setup_inputs(seed: int = 0) -> dict:
    key = jax.random.key(seed)
    ks = jax.random.split(key, 32)
    L, D = DEPTH, D_MODEL

    def nrm(k, shape, scale):
        return jax.random.normal(k, shape, jnp.float32) * scale

    a_c = jax.random.uniform(ks[10], (L, LRU_WIDTH), jnp.float32, 0.9, 0.999)
    s_base = a_c ** (1.0 / LRU_C)
    lam = jnp.log(s_base) - jnp.log1p(-s_base)
    return {
        'x': nrm(ks[0], (BATCH, SEQ, D), 1.0),
        'p': nrm(ks[1], (L, BATCH, SEQ, PLE_DIM), 1.0),
        'norm_mix_g': 1.0 + nrm(ks[2], (L, D), 0.01),
        'w_in': nrm(ks[3], (L, D, IN_WIDTH), D ** -0.5),
        'b_in': nrm(ks[4], (L, IN_WIDTH), 0.01),
        'conv_w': nrm(ks[5], (L, CONV_WIDTH, LRU_WIDTH), CONV_WIDTH ** -0.5),
        'conv_b': nrm(ks[6], (L, LRU_WIDTH), 0.01),
        'w_rg_a': nrm(ks[7], (L, LRU_BLOCKS, LRU_BLOCK_WIDTH, LRU_BLOCK_WIDTH), LRU_BLOCK_WIDTH ** -0.5),
        'b_rg_a': nrm(ks[8], (L, LRU_BLOCKS, LRU_BLOCK_WIDTH), 0.01),
        'w_rg_x': nrm(ks[9], (L, LRU_BLOCKS, LRU_BLOCK_WIDTH, LRU_BLOCK_WIDTH), LRU_BLOCK_WIDTH ** -0.5),
        'b_rg_x': nrm(ks[11], (L, LRU_BLOCKS, LRU_BLOCK_WIDTH), 0.01),
        'lru_lambda': lam,
        'attn_sinks': nrm(ks[12], (L, N_Q_HEADS), 1.0),
        'w_attn_proj': nrm(ks[13], (L, Q_WIDTH, D), Q_WIDTH ** -0.5),
        'w_lru_proj': nrm(ks[14], (L, LRU_WIDTH, D), LRU_WIDTH ** -0.5),
        'w_out': nrm(ks[15], (L, D, D), D ** -0.5),
        'norm_ffn_g': 1.0 + nrm(ks[16], (L, D), 0.01),
        'w_router': nrm(ks[17], (L, D, N_EXPERTS), D ** -0.5),
        'b_router': nrm(ks[18], (L, N_EXPERTS), 0.01),
        'w_mlp1': nrm(ks[19], (L, N_EXPERTS, D, 2 * D_EXPERT), D ** -0.5),
        'b_mlp1': nrm(ks[20], (L, N_EXPERTS, 2 * D_EXPERT), 0.01),
        'w_mlp2': nrm(ks[21], (L, N_EXPERTS, D_EXPERT, D), D_EXPERT ** -0.5),
        'b_mlp2': nrm(ks[22], (L, N_EXPERTS, D), 0.01),
        'norm_ple_g': 1.0 + nrm(ks[23], (L, D), 0.01),
        'w_ple': nrm(ks[24], (L, PLE_DIM, D), PLE_DIM ** -0.5),
        'w_ple_gate': nrm(ks[25], (L, D, D), D ** -0.5),
        'norm_final_g': 1.0 + nrm(ks[26], (D,), 0.01),
    }


def reference(x, p, norm_mix_g, w_in, b_in, conv_w, conv_b, w_rg_a, b_rg_a, w_rg_x, b_rg_x,
              lru_lambda, attn_sinks, w_attn_proj, w_lru_proj, w_out, norm_ffn_g, w_router,
              b_router, w_mlp1, b_mlp1, w_mlp2, b_mlp2, norm_ple_g, w_ple, w_ple_gate,
              norm_final_g):
    h = x
    for l in range(DEPTH):
        xn = rmsnorm(h, norm_mix_g[l])
        z = xn @ w_in[l] + b_in[l]
        q, k, v, u, lru_gate, g_attn, g_lru = jnp.split(z, IN_CUTS, axis=-1)
        y_attn = sliding_window_attention(q, k, v, attn_sinks[l])
        y_lru = rglru_branch(u, lru_gate, conv_w[l], conv_b[l], w_rg_a[l], b_rg_a[l],
                             w_rg_x[l], b_rg_x[l], lru_lambda[l])
        merged = (jax.nn.sigmoid(g_attn) * (y_attn @ w_attn_proj[l])
                  + jax.nn.sigmoid(g_lru) * (y_lru @ w_lru_proj[l]))
        h = h + merged @ w_out[l]
        h = h + moe(rmsnorm(h, norm_ffn_g[l]), w_router[l], b_router[l],
                    w_mlp1[l], b_mlp1[l], w_mlp2[l], b_mlp2[l])
        ple_gate = jax.nn.sigmoid(rmsnorm(h, norm_ple_g[l]) @ w_ple_gate[l])
        h = h + ple_gate * (p[l] @ w_ple[l])
    return rmsnorm(h, norm_final_g)
```

```python
import contextlib
import numpy as np
import concourse.bass as bass
import concourse.mybir as mybir
from concourse.bass_utils import run_bass_kernel_spmd

F32 = mybir.dt.float32
BF16 = mybir.dt.bfloat16
I32 = mybir.dt.int32
ALU = mybir.AluOpType
AF = mybir.ActivationFunctionType

D = 2048
KC = 16
NT = 512
TOWN = 2048
NTILE = TOWN // NT
NE = 32
CAP = 352
NJT = 3
JR = (128, 128, 96)
BIG = 65536.0
EPS = 1e-6
NCORES = 8
STAGE = 99

C_Q = 0
C_K = 16
C_LRU = 20
C_GATE = 52
NWIN = 84

O_IDF = 0
O_ECAP = 128
O_BROUT = 160
O_BV = 192
O_BIN = 448
O_BRG = O_BIN + NWIN
O_CONVW = O_BRG + 32
O_CONVB = O_CONVW + 64
O_LAM = O_CONVB + 16
O_GAIN = O_LAM + 16
O_SINK = O_GAIN + 64
O_B1 = O_SINK + 16
NCF = O_B1 + 1024
B_ID = 0
B_LT = 128
B_ONES = 256
B_MB4 = 384
B_OLO = 896
B_OHI = 1024
NCB = 1152

ENGS = ("pe", "act", "dve", "pool", "sp")


class Op:
    __slots__ = ("eng", "fn", "deps", "is_dma", "sem", "cnt", "signal", "sig_idx")


class Prog:
    def __init__(self, nc, n_dma_sems=8):
        self.nc = nc
        self.ops = {e: [] for e in ENGS}
        self.last_w = {}
        self.readers = {}
        self.n_dma_sems = n_dma_sems
        self.dma_rr = {e: 0 for e in ENGS}
        self.dma_last = {}
        self.dma_cnt = {}

    def add(self, eng, fn, r=(), w=(), dma=False, extra=()):
        op = Op()
        op.eng = eng; op.fn = fn; op.is_dma = dma; op.signal = False; op.sig_idx = 0
        op.sem = None; op.cnt = 0
        deps = list(extra)
        for t in r:
            x = self.last_w.get(t)
            if x is not None:
                deps.append(x)
        for t in w:
            x = self.last_w.get(t)
            if x is not None:
                deps.append(x)
            deps.extend(self.readers.get(t, ()))
        if dma:
            slot = self.dma_rr[eng] % self.n_dma_sems
            self.dma_rr[eng] += 1
            key = (eng, slot)
            prev = self.dma_last.get(key)
            if prev is not None:
                deps.append(prev)
            self.dma_last[key] = op
            self.dma_cnt[key] = self.dma_cnt.get(key, 0) + 16
            op.sem = key
            op.cnt = self.dma_cnt[key]
        op.deps = deps
        for t in r:
            self.readers.setdefault(t, []).append(op)
        for t in w:
            self.last_w[t] = op
            self.readers[t] = []
        self.ops[eng].append(op)
        return op

    def all_tails(self):
        tails = []
        for e in ENGS:
            for op in reversed(self.ops[e]):
                if not op.is_dma:
                    tails.append(op)
                    break
        tails.extend(self.dma_last.values())
        return tails

    def setup(self, st):
        nc = self.nc
        self.esem = {e: st.enter_context(nc.semaphore("s_" + e)) for e in ENGS}
        self.dsem = {}
        for e in ("sp", "pool", "act"):
            for i in range(self.n_dma_sems):
                self.dsem[(e, i)] = st.enter_context(nc.semaphore("d_%s_%d" % (e, i)))
        self.emitted = {e: 0 for e in ENGS}
        self.sigc = {e: 0 for e in ENGS}
        self.waited_e = {e: {x: 0 for x in ENGS} for e in ENGS}
        self.waited_d = {e: {} for e in ENGS}
        self.done_ops = set()

    def flush(self, final=False):
        nc = self.nc
        pend = {e: self.ops[e][self.emitted[e]:] for e in ENGS}
        for e in ENGS:
            for op in pend[e]:
                for d in op.deps:
                    if not d.is_dma and not (d.eng == e and e == "pe") and id(d) not in self.done_ops:
                        d.signal = True
        for e in ENGS:
            for op in pend[e]:
                if (not op.is_dma) and op.signal:
                    self.sigc[e] += 1
                    op.sig_idx = self.sigc[e]
        prog = self
        esem, dsem = self.esem, self.dsem
        with nc.Block() as block:
            def run(e, h):
                if e == "pool":
                    prog.bc_reg = h.to_reg(NE * CAP - 1)
                waited_e = prog.waited_e[e]
                waited_d = prog.waited_d[e]
                for op in pend[e]:
                    need_e = {}
                    need_d = {}
                    for d in op.deps:
                        if d.is_dma:
                            if waited_d.get(d.sem, 0) < d.cnt:
                                need_d[d.sem] = max(need_d.get(d.sem, 0), d.cnt)
                        else:
                            if d.eng == e and e == "pe":
                                continue
                            if d.sig_idx == 0:
                                assert id(d) in prog.done_ops, "unsignalled dep in same phase"
                                continue
                            if waited_e[d.eng] < d.sig_idx:
                                need_e[d.eng] = max(need_e.get(d.eng, 0), d.sig_idx)
                    for x, v in need_e.items():
                        h.wait_ge(esem[x], v); waited_e[x] = v
                    for k, v in need_d.items():
                        h.wait_ge(dsem[k], v); waited_d[k] = v
                    ins = op.fn(h)
                    if op.is_dma:
                        ins.then_inc(dsem[op.sem], 16)
                    elif op.signal:
                        ins.then_inc(esem[e], 1)
                if final:
                    for key, cnt in prog.dma_cnt.items():
                        if key[0] == e:
                            h.wait_ge(dsem[key], cnt)

            @block.sync
            def _(h):
                run("sp", h)

            @block.scalar
            def _(h):
                run("act", h)

            @block.vector
            def _(h):
                run("dve", h)

            @block.gpsimd
            def _(h):
                run("pool", h)

            @block.tensor
            def _(h):
                run("pe", h)
        for e in ENGS:
            for op in pend[e]:
                self.done_ops.add(id(op))
            self.emitted[e] = len(self.ops[e])


def build_nc(debug=False, n_prefix=NTILE, n_own=NTILE, n_exp=NE, final=True):
    NEd = max(1, n_exp)
    nc = bass.Bass("TRN2", target_bir_lowering=False)
    P = Prog(nc)
    A = P.add

    def din(name, shape, dt=F32):
        return nc.dram_tensor(name, list(shape), dt, kind="ExternalInput").ap()

    xT = din("xT", [128, KC, 2 * TOWN])
    pT = din("pT", [128, 2, TOWN])
    cc = din("cc", [128, 516])
    cbf_d = din("cbf", [128, NCB])
    cf_d = din("cf", [128, NCF])
    win = din("win", [NWIN, 128, KC, 128])
    wv_d = din("wv", [128, KC, 256])
    wproj = din("wproj", [4, KC, 128, KC, 128])
    wrg_d = din("wrg", [8, 128, 2, 2, 256])
    wrouter_d = din("wrouter", [128, KC, NE])
    wple_d = din("wple", [128, 2, D])
    w1_d = din("w1", [NEd, 32, 128, KC, 128])
    w2_d = din("w2", [NEd, 4, 128, KC, 512])
    b2_d = din("b2", [NEd, D])
    outT = nc.dram_tensor("outT", [128, KC, TOWN], F32, kind="ExternalOutput").ap()
    skind = "ExternalOutput" if debug else "Internal"
    hmid = nc.dram_tensor("hmid", [128, KC, TOWN], F32, kind=skind).ap()
    xg = nc.dram_tensor("xg", [NE * CAP, D], BF16, kind="Internal").ap()
    yg = nc.dram_tensor("yg", [NE * CAP, D], F32, kind="Internal").ap()

    with contextlib.ExitStack() as gst:
        def sbg(name, shape, dt):
            return gst.enter_context(nc.sbuf_tensor(name, list(shape), dt))

        P.setup(gst)
        banks = [gst.enter_context(nc.psum_tensor("bank%d" % i, [128, 512], F32)) for i in range(8)]
        bank_rr = [0]

        def nb():
            i = bank_rr[0] % 8
            bank_rr[0] += 1
            return banks[i], ("bank", i)

        cbf = sbg("cbf_t", [128, NCB], BF16)
        cf = sbg("cf_t", [128, NCF], F32)
        mb40 = sbg("mb40", [128, 512], BF16)
        flags = sbg("flags", [128, 4], F32)
        wbufs = [sbg("wbuf%d" % i, [128, KC, 128], BF16) for i in range(4)]
        wrr = [0]
        dest_f = sbg("dest_f", [128, 64], F32)
        dest_i = sbg("dest_i", [128, 64], I32)
        w4 = sbg("w4", [128, 64], F32)
        bsc = sbg("bsc", [128, 8], F32)

        ident_bf = cbf[:, B_ID:B_ID + 128]
        lt_bf = cbf[:, B_LT:B_LT + 128]
        ones_bf = cbf[:, B_ONES:B_ONES + 128]
        mb4 = cbf[:, B_MB4:B_MB4 + 512]
        oneslo = cbf[:, B_OLO:B_OLO + 128]
        oneshi = cbf[:, B_OHI:B_OHI + 128]
        ident_f = cf[:, O_IDF:O_IDF + 128]

        A("pool", lambda h: h.dma_start(out=cbf[:], in_=cbf_d), w=["cbf"], dma=True)
        A("pool", lambda h: h.dma_start(out=mb40[:], in_=cc[:, 0:512]), w=["mb40"], dma=True)
        A("sp", lambda h: h.dma_start(out=cf[:], in_=cf_d), w=["cf"], dma=True)
        A("sp", lambda h: h.dma_start(out=flags[:], in_=cc[:, 512:516]), w=["flags"], dma=True)

        def wstream(src):
            i = wrr[0] % len(wbufs)
            wrr[0] += 1
            t = wbufs[i]
            A("pool", lambda h: h.dma_start(out=t[:], in_=src), w=[("wb", i)], dma=True)
            return t, ("wb", i)

        bar_n = [0]

        def barrier():
            tails = P.all_tails()
            n = bar_n[0]
            bar_n[0] += 1
            for e in ("act", "dve", "pool"):
                col = {"act": 0, "dve": 1, "pool": 2}[e]
                if e == "act":
                    A(e, lambda h, col=col: h.memzero(bsc[:, col:col + 1]), w=[("bsc", e)], extra=tails)
                else:
                    A(e, lambda h, col=col: h.memset(bsc[:, col:col + 1], 0.0), w=[("bsc", e)], extra=tails)
            b, bt = nb()
            A("pe", lambda h: h.matmul(b[0:1, 0:2], lhsT=ones_bf[0:1, 0:1], rhs=ones_bf[0:1, 0:2], start=True, stop=True),
              r=["cbf"], w=[bt], extra=tails)
            A("sp", lambda h: h.dma_start(out=bsc[:, 4:8], in_=cc[:, 512:516]), w=[("bsc", "sp")], dma=True, extra=tails)

        with contextlib.ExitStack() as st:
            def sb(name, shape, dt):
                return st.enter_context(nc.sbuf_tensor(name, list(shape), dt))

            hT = sb("hT", [128, KC, NT], F32)
            xn = sb("xn", [128, KC, NT], BF16)
            qT = sb("qT", [128, KC, NT], BF16)
            kT = sb("kT", [128, 4, NT + 128], BF16)
            vlo = sb("vlo", [128, 5, 4, 128], BF16)
            vhi = sb("vhi", [128, 5, 4, 128], BF16)
            yat = sb("yat", [128, KC, NT], BF16)
            ylr = sb("ylr", [128, KC, NT], BF16)
            wv = sb("wv_t", [128, KC, 256], BF16)
            wrgt = [sb("wrgt%d" % i, [128, 2, 2, 256], BF16) for i in range(2)]
            wrt = sb("wrt", [128, KC, NE], BF16)
            sqb = [sb("sqb%d" % i, [128, NT], BF16) for i in range(2)]
            rs = sb("rs", [128, NT], F32)
            rstd = sb("rstd", [128, NT], F32)
            u = sb("u", [128, 2, NT + 3], F32)
            uc = sb("uc", [128, 2, NT], F32)
            ucb = sb("ucb", [128, 2, NT], BF16)
            uh = sb("uh", [128, KC, 3], F32)
            hstate = sb("hstate", [128, KC], F32)
            rt = [sb("rt%d" % i, [128, NT], F32) for i in range(2)]
            igt = [sb("igt%d" % i, [128, NT], F32) for i in range(2)]
            at = [sb("at%d" % i, [128, NT], F32) for i in range(2)]
            a2t = [sb("a2t%d" % i, [128, NT], F32) for i in range(2)]
            hseq = [sb("hseq%d" % i, [128, NT], F32) for i in range(2)]
            gt = [sb("gt%d" % i, [128, NT], F32) for i in range(2)]
            t1 = [sb("t1%d" % i, [128, NT], F32) for i in range(2)]
            pm = [sb("pm%d" % i, [128, 512], BF16) for i in range(3)]
            dent = [sb("dent%d" % i, [128, 128], F32) for i in range(2)]
            rect = [sb("rect%d" % i, [128, 128], F32) for i in range(2)]
            nsp8 = sb("nsp8", [128, KC], F32)
            nsp16 = sb("nsp16", [128, KC], F32)
            esink = sb("esink", [128, KC], F32)
            hbrg = sb("hbrg", [128, 32], F32)
            hbin = sb("hbin", [128, NWIN], F32)
            xtok = sb("xtok", [128, D], BF16)
            lg = sb("lg", [128, NE], F32)
            top8 = sb("top8", [128, 8], F32)
            ntop = sb("ntop", [128, 1], F32)
            mk = sb("mk", [128, NE], F32)
            mkb = sb("mkb", [128, NE], BF16)
            rk = sb("rk", [128, NE], F32)
            okr = sb("okr", [128, NE], F32)
            cntb = sb("cntb", [128, NE], F32)
            ex4 = sb("ex4", [128, 4], F32)
            s4 = sb("s4", [128, 1], F32)
            junk = sb("junk", [128, NE], F32)

            A("pool", lambda h: h.dma_start(out=wv[:], in_=wv_d), w=["wv"], dma=True)
            A("pool", lambda h: h.dma_start(out=wrt[:], in_=wrouter_d), w=["wrt"], dma=True)
            A("dve", lambda h: h.memset(vlo[:], 0.0), w=["vlo"])
            A("dve", lambda h: h.memset(vhi[:], 0.0), w=["vhi"])
            A("dve", lambda h: h.memset(kT[:], 0.0), w=["kT"])
            A("dve", lambda h: h.memset(uh[:], 0.0), w=["uh"])
            A("dve", lambda h: h.memset(hstate[:], 0.0), w=["hstate"])
            A("dve", lambda h: h.memset(cntb[:], 0.0), w=["cntb"])
            A("act", lambda h: h.activation(out=nsp8[:], in_=cf[:, O_LAM:O_LAM + 16], func=AF.Exp, scale=-1.0),
              r=["cf"], w=["nsp8"])
            A("act", lambda h: h.activation(out=nsp8[:], in_=nsp8[:], func=AF.Ln, bias=1.0), r=["nsp8"], w=["nsp8"])
            A("dve", lambda h: h.tensor_scalar(out=nsp16[:], in0=nsp8[:], scalar1=-4.0, scalar2=None, op0=ALU.mult),
              r=["nsp8"], w=["nsp16"])
            A("dve", lambda h: h.tensor_scalar(out=hbrg[:], in0=cf[:, O_BRG:O_BRG + 32], scalar1=0.5, scalar2=None, op0=ALU.mult),
              r=["cf"], w=["hbrg"])
            A("dve", lambda h: h.tensor_scalar(out=hbin[:], in0=cf[:, O_BIN:O_BIN + NWIN], scalar1=0.5, scalar2=None, op0=ALU.mult),
              r=["cf"], w=["hbin"])
            A("dve", lambda h: h.tensor_scalar(out=nsp8[:], in0=nsp8[:], scalar1=-8.0, scalar2=None, op0=ALU.mult),
              r=["nsp8", "nsp16"], w=["nsp8"])
            A("act", lambda h: h.activation(out=esink[:], in_=cf[:, O_SINK:O_SINK + 16], func=AF.Exp),
              r=["cf"], w=["esink"])

            def rmsnorm_to_xn(gidx, dst, dst_tok):
                b, bt = nb()
                for k in range(KC):
                    sq = sqb[k % 2]
                    A("act", lambda h, k=k, sq=sq: h.activation(out=sq[:], in_=hT[:, k, :], func=AF.Square),
                      r=[("h", k)], w=[("sq", k % 2)])
                    A("pe", lambda h, k=k, sq=sq: h.matmul(b[:, :], lhsT=ones_bf, rhs=sq[:], start=(k == 0), stop=(k == KC - 1)),
                      r=[("sq", k % 2), "cbf"], w=[bt])
                A("act", lambda h: h.activation(out=rs[:], in_=b[:, :], func=AF.Sqrt, scale=1.0 / D, bias=EPS),
                  r=[bt], w=["rs"])
                A("dve", lambda h: h.reciprocal(out=rstd[:], in_=rs[:]), r=["rs"], w=["rstd"])
                for k in range(KC):
                    A("dve", lambda h, k=k: h.scalar_tensor_tensor(
                        out=dst[:, k, :], in0=hT[:, k, :], scalar=cf[:, O_GAIN + gidx * 16 + k:O_GAIN + gidx * 16 + k + 1],
                        in1=rstd[:], op0=ALU.mult, op1=ALU.mult),
                      r=[("h", k), "rstd", "cf"], w=[(dst_tok, k)])

            def win_chunk(cidx):
                wt, wtok = wstream(win[cidx])
                b, bt = nb()
                for k in range(KC):
                    A("pe", lambda h, k=k: h.matmul(b[:, :], lhsT=wt[:, k, :], rhs=xn[:, k, :], start=(k == 0), stop=(k == KC - 1)),
                      r=[wtok, ("xn", k)], w=[bt])
                return b, bt

            def bias_col(cidx):
                return cf[:, O_BIN + cidx:O_BIN + cidx + 1]

            def kv_chunks():
                for kvh in range(4):
                    b, bt = win_chunk(C_K + kvh)
                    A("act", lambda h, b=b, kvh=kvh: h.activation(out=kT[:, kvh, 128:128 + NT], in_=b[:, :], func=AF.Identity,
                                                                  bias=bias_col(C_K + kvh)),
                      r=[bt, "cf"], w=["kT"])
                for half in range(2):
                    b, bt = nb()
                    for tb2 in range(2):
                        tb = half * 2 + tb2
                        for k in range(KC):
                            A("pe", lambda h, k=k, tb=tb, tb2=tb2, b=b: h.matmul(
                                b[:, tb2 * 256:(tb2 + 1) * 256], lhsT=xn[:, k, tb * 128:(tb + 1) * 128], rhs=wv[:, k, :],
                                start=(k == 0), stop=(k == KC - 1)),
                              r=[("xn", k), "wv"], w=[bt])
                    for tb2 in range(2):
                        tb = half * 2 + tb2
                        src = b[:, tb2 * 256:(tb2 + 1) * 256].rearrange("p (a c) -> p a c", c=64)
                        bvv = cf[:, O_BV:O_BV + 256].rearrange("p (a c) -> p a c", c=64)
                        A("dve", lambda h, tb=tb, src=src, bvv=bvv: h.tensor_tensor(out=vlo[:, 1 + tb, :, 0:64], in0=src, in1=bvv, op=ALU.add),
                          r=[bt, "cf"], w=["vlo"])
                        A("dve", lambda h, tb=tb, src=src, bvv=bvv: h.tensor_tensor(out=vhi[:, 1 + tb, :, 64:128], in0=src, in1=bvv, op=ALU.add),
                          r=[bt, "cf"], w=["vhi"])

            def kv_halo():
                A("act", lambda h: h.activation(out=kT[:, :, 0:128], in_=kT[:, :, NT:NT + 128], func=AF.Copy), r=["kT"], w=["kT"])
                A("dve", lambda h: h.tensor_copy(out=vlo[:, 0, :, :], in_=vlo[:, 4, :, :]), r=["vlo"], w=["vlo"])
                A("dve", lambda h: h.tensor_copy(out=vhi[:, 0, :, :], in_=vhi[:, 4, :, :]), r=["vhi"], w=["vhi"])

            def lru_block(n, prefix, first_prefix, first_own):
                wg = wrgt[n % 2]
                A("pool", lambda h: h.dma_start(out=wg[:], in_=wrg_d[n]), w=[("wrg", n % 2)], dma=True)
                for j in range(2):
                    ci = C_LRU + 4 * n + j
                    b, bt = win_chunk(ci)
                    A("act", lambda h, b=b, j=j, ci=ci: h.activation(out=u[:, j, 3:3 + NT], in_=b[:, :], func=AF.Identity, bias=bias_col(ci)),
                      r=[bt, "cf"], w=[("u", j)])
                A("dve", lambda h: h.tensor_copy(out=u[:, :, 0:3], in_=uh[:, 2 * n:2 * n + 2, :]), r=["uh"], w=[("u", 0), ("u", 1)])
                A("dve", lambda h: h.tensor_copy(out=uh[:, 2 * n:2 * n + 2, :], in_=u[:, :, NT:NT + 3]), r=[("u", 0), ("u", 1)], w=["uh"])
                for j in range(2):
                    c = 2 * n + j
                    A("act", lambda h, j=j, c=c: h.activation(out=uc[:, j, :], in_=u[:, j, 0:NT], func=AF.Identity,
                                                             scale=cf[:, O_CONVW + c:O_CONVW + c + 1], bias=cf[:, O_CONVB + c:O_CONVB + c + 1]),
                      r=[("u", j), "cf"], w=[("uc", j)])
                for tap in range(1, 4):
                    for j in range(2):
                        c = 2 * n + j
                        A("dve", lambda h, j=j, c=c, tap=tap: h.scalar_tensor_tensor(
                            out=uc[:, j, :], in0=u[:, j, tap:tap + NT], scalar=cf[:, O_CONVW + tap * 16 + c:O_CONVW + tap * 16 + c + 1],
                            in1=uc[:, j, :], op0=ALU.mult, op1=ALU.add),
                          r=[("u", j), ("uc", j), "cf"], w=[("uc", j)])
                for j in range(2):
                    A("act", lambda h, j=j: h.activation(out=ucb[:, j, :], in_=uc[:, j, :], func=AF.Copy), r=[("uc", j)], w=[("ucb", j)])
                yield
                gb_ = {}
                for j in range(2):
                    for ax in range(2):
                        bb, btt = nb()
                        gb_[(j, ax)] = (bb, btt)
                        for i in range(2):
                            A("pe", lambda h, ax=ax, bb=bb, i=i, j=j: h.matmul(
                                bb[:, :], lhsT=wg[:, ax, i, j * 128:(j + 1) * 128], rhs=ucb[:, i, :], start=(i == 0), stop=(i == 1)),
                              r=[("wrg", n % 2), ("ucb", i)], w=[btt])
                for j in range(2):
                    c = 2 * n + j
                    br, btr = gb_[(j, 0)]
                    bx, btx = gb_[(j, 1)]
                    A("act", lambda h, br=br, c=c, j=j: h.activation(out=rt[j][:], in_=br[:, :], func=AF.Tanh, scale=0.5, bias=hbrg[:, c:c + 1]),
                      r=[btr, "hbrg"], w=[("rt", j)])
                    A("act", lambda h, bx=bx, c=c, j=j: h.activation(out=igt[j][:], in_=bx[:, :], func=AF.Tanh, scale=0.5, bias=hbrg[:, 16 + c:16 + c + 1]),
                      r=[btx, "hbrg"], w=[("igt", j)])
                for j in range(2):
                    c = 2 * n + j
                    A("act", lambda h, c=c, j=j: h.activation(out=at[j][:], in_=rt[j][:], func=AF.Exp, scale=nsp16[:, c:c + 1], bias=nsp16[:, c:c + 1]),
                      r=[("rt", j), "nsp16"], w=[("at", j)])
                    A("act", lambda h, c=c, j=j: h.activation(out=a2t[j][:], in_=rt[j][:], func=AF.Exp, scale=nsp8[:, c:c + 1], bias=nsp8[:, c:c + 1]),
                      r=[("rt", j), "nsp8"], w=[("a2t", j)])
                for j in range(2):
                    A("act", lambda h, j=j: h.activation(out=a2t[j][:], in_=a2t[j][:], func=AF.Sqrt, scale=-1.0, bias=1.0), r=[("a2t", j)], w=[("a2t", j)])
                for j in range(2):
                    if first_prefix:
                        A("dve", lambda h, j=j: h.memset(a2t[j][:, 0:1], 1.0), r=[("a2t", j)], w=[("a2t", j)])
                    if first_own:
                        A("dve", lambda h, j=j: h.tensor_scalar(out=a2t[j][:, 0:1], in0=a2t[j][:, 0:1], scalar1=flags[:, 1:2], scalar2=flags[:, 2:3],
                                                                op0=ALU.mult, op1=ALU.add), r=[("a2t", j), "flags"], w=[("a2t", j)])
                for j in range(2):
                    A("dve", lambda h, j=j: h.scalar_tensor_tensor(out=igt[j][:], in0=igt[j][:], scalar=1.0, in1=uc[:, j, :], op0=ALU.add, op1=ALU.mult),
                      r=[("igt", j), ("uc", j)], w=[("igt", j)])
                for j in range(2):
                    A("dve", lambda h, j=j: h.scalar_tensor_tensor(out=igt[j][:], in0=igt[j][:], scalar=0.5, in1=a2t[j][:], op0=ALU.mult, op1=ALU.mult),
                      r=[("igt", j), ("a2t", j)], w=[("igt", j)])
                for j in range(2):
                    c = 2 * n + j
                    A("dve", lambda h, c=c, j=j: h.tensor_tensor_scan(out=hseq[j][:], data0=at[j][:], data1=igt[j][:], initial=hstate[:, c:c + 1],
                                                                      op0=ALU.mult, op1=ALU.add), r=[("at", j), ("igt", j), "hstate"], w=[("hseq", j)])
                for j in range(2):
                    c = 2 * n + j
                    A("dve", lambda h, c=c, j=j: h.tensor_copy(out=hstate[:, c:c + 1], in_=hseq[j][:, NT - 1:NT]), r=[("hseq", j)], w=["hstate"])
                yield
                if not prefix:
                    for j in range(2):
                        ci = C_LRU + 4 * n + 2 + j
                        b, bt = win_chunk(ci)
                        A("act", lambda h, b=b, ci=ci, j=j: h.activation(out=gt[j][:], in_=b[:, :], func=AF.Identity, bias=bias_col(ci)),
                          r=[bt, "cf"], w=[("gt", j)])
                        A("act", lambda h, j=j: h.activation(out=t1[j][:], in_=gt[j][:], func=AF.Square), r=[("gt", j)], w=[("t1", j)])
                    for j in range(2):
                        A("dve", lambda h, j=j: h.tensor_scalar(out=t1[j][:], in0=t1[j][:], scalar1=0.044715, scalar2=1.0, op0=ALU.mult, op1=ALU.add),
                          r=[("t1", j)], w=[("t1", j)])
                    for j in range(2):
                        A("dve", lambda h, j=j: h.tensor_tensor(out=t1[j][:], in0=t1[j][:], in1=gt[j][:], op=ALU.mult), r=[("t1", j), ("gt", j)], w=[("t1", j)])
                    for j in range(2):
                        A("act", lambda h, j=j: h.activation(out=t1[j][:], in_=t1[j][:], func=AF.Tanh, scale=0.7978845608028654), r=[("t1", j)], w=[("t1", j)])
                    for j in range(2):
                        A("dve", lambda h, j=j: h.scalar_tensor_tensor(out=t1[j][:], in0=t1[j][:], scalar=1.0, in1=gt[j][:], op0=ALU.add, op1=ALU.mult),
                          r=[("t1", j), ("gt", j)], w=[("t1", j)])
                    for j in range(2):
                        c = 2 * n + j
                        A("dve", lambda h, c=c, j=j: h.scalar_tensor_tensor(out=ylr[:, c, :], in0=t1[j][:], scalar=0.5, in1=hseq[j][:], op0=ALU.mult, op1=ALU.mult),
                          r=[("t1", j), ("hseq", j)], w=[("ylr", c)])
                    yield

            def attention(first_own):
                iters = [(qb, c) for qb in range(4) for c in range(KC)]
                sbanks = {}

                def s_stage(i):
                    qb, c = iters[i]
                    kvh = c // 4
                    p = pm[i % 3]
                    use0 = first_own and qb == 0
                    for hh in range(2):
                        lo, hi = hh * 64, hh * 64 + 64
                        b, bt = nb()
                        if use0:
                            A("pe", lambda h, b=b: h.matmul(b[:, 0:256], lhsT=ident_bf, rhs=mb40[:, 0:256], start=True, stop=False),
                              r=["cbf", "mb40"], w=[bt])
                        else:
                            A("pe", lambda h, b=b: h.matmul(b[:, 0:256], lhsT=ident_bf, rhs=mb4[:, 0:256], start=True, stop=False),
                              r=["cbf"], w=[bt])
                        for pc in range(2):
                            col = pc * 128
                            kb = qb + pc
                            A("pe", lambda h, b=b, lo=lo, hi=hi, col=col, kb=kb, pc=pc: h.matmul(
                                b[:, col:col + 128], lhsT=kT[lo:hi, kvh, kb * 128:(kb + 1) * 128], rhs=qT[lo:hi, c, qb * 128:(qb + 1) * 128],
                                start=False, stop=(pc == 1)),
                              r=["kT", ("q", c)], w=[bt])
                        A("act", lambda h, b=b, hh=hh: h.activation(out=p[:, hh * 256:(hh + 1) * 256], in_=b[:, 0:256], func=AF.Exp, scale=0.125),
                          r=[bt], w=[("pm", i % 3)])

                def pv_stage(i):
                    qb, c = iters[i]
                    kvh = c // 4
                    p = pm[i % 3]
                    b, bt = nb()
                    seq = [(vlo, qb, 0), (vlo, qb + 1, 1), (vhi, qb, 2), (vhi, qb + 1, 3)]
                    for n_, (vt, blk, pc) in enumerate(seq):
                        A("pe", lambda h, vt=vt, blk=blk, pc=pc, n_=n_: h.matmul(
                            b[:, 0:128], lhsT=vt[:, blk, kvh, :], rhs=p[:, pc * 128:(pc + 1) * 128], start=(n_ == 0), stop=(n_ == 3)),
                          r=["vlo", "vhi", ("pm", i % 3)], w=[bt])
                    seq2 = [(oneslo, 0), (oneslo, 1), (oneshi, 2), (oneshi, 3)]
                    for n_, (ot, pc) in enumerate(seq2):
                        A("pe", lambda h, ot=ot, pc=pc, n_=n_: h.matmul(
                            b[:, 128:256], lhsT=ot, rhs=p[:, pc * 128:(pc + 1) * 128], start=(n_ == 0), stop=(n_ == 3)),
                          r=["cbf", ("pm", i % 3)], w=[bt])
                    de = dent[i % 2]
                    re = rect[i % 2]
                    A("dve", lambda h: h.tensor_scalar(out=de[:], in0=b[:, 128:256], scalar1=esink[:, c:c + 1], scalar2=None, op0=ALU.add),
                      r=[bt, "esink"], w=[("dent", i % 2)])
                    A("dve", lambda h: h.reciprocal(out=re[:], in_=de[:]), r=[("dent", i % 2)], w=[("rect", i % 2)])
                    A("dve", lambda h: h.tensor_tensor(out=yat[:, c, qb * 128:(qb + 1) * 128], in0=b[:, 0:128], in1=re[:], op=ALU.mult),
                      r=[bt, ("rect", i % 2)], w=[("yat", c)])

                n_it = len(iters)
                s_stage(0)
                s_stage(1)
                for i in range(n_it):
                    pv_stage(i)
                    if i + 2 < n_it:
                        s_stage(i + 2)
                    if i % 2 == 1:
                        yield

            def merge_and_out(ti):
                mg = qT
                for j in range(KC):
                    jj = j % 2
                    ga, gl, m1, m2 = rt[jj], igt[jj], at[jj], a2t[jj]
                    wa, wat = wstream(wproj[0, j])
                    ba, bat = nb()
                    for k in range(KC):
                        A("pe", lambda h, k=k, wa=wa, ba=ba: h.matmul(ba[:, :], lhsT=wa[:, k, :], rhs=yat[:, k, :], start=(k == 0), stop=(k == KC - 1)),
                          r=[wat, ("yat", k)], w=[bat])
                    wb_, wbt = wstream(wproj[1, j])
                    bb, bbt = nb()
                    for k in range(KC):
                        A("pe", lambda h, k=k, wb_=wb_, bb=bb: h.matmul(bb[:, :], lhsT=wb_[:, k, :], rhs=ylr[:, k, :], start=(k == 0), stop=(k == KC - 1)),
                          r=[wbt, ("ylr", k)], w=[bbt])
                    cga = C_GATE + 2 * j
                    bga, bgat = win_chunk(cga)
                    bgl, bglt = win_chunk(cga + 1)
                    A("act", lambda h, bga=bga, cga=cga, ga=ga: h.activation(out=ga[:], in_=bga[:, :], func=AF.Tanh, scale=0.5, bias=hbin[:, cga:cga + 1]),
                      r=[bgat, "hbin"], w=[("rt", jj)])
                    A("act", lambda h, bgl=bgl, cga=cga, gl=gl: h.activation(out=gl[:], in_=bgl[:, :], func=AF.Tanh, scale=0.5, bias=hbin[:, cga + 1:cga + 2]),
                      r=[bglt, "hbin"], w=[("igt", jj)])
                    A("dve", lambda h, ba=ba, ga=ga, m1=m1: h.scalar_tensor_tensor(out=m1[:], in0=ga[:], scalar=1.0, in1=ba[:, :], op0=ALU.add, op1=ALU.mult),
                      r=[bat, ("rt", jj)], w=[("at", jj)])
                    A("dve", lambda h, bb=bb, gl=gl, m2=m2: h.scalar_tensor_tensor(out=m2[:], in0=gl[:], scalar=1.0, in1=bb[:, :], op0=ALU.add, op1=ALU.mult),
                      r=[bbt, ("igt", jj)], w=[("a2t", jj)])
                    A("dve", lambda h, j=j, m1=m1, m2=m2: h.tensor_tensor(out=mg[:, j, :], in0=m1[:], in1=m2[:], op=ALU.add),
                      r=[("at", jj), ("a2t", jj)], w=[("q", j)])
                for j in range(KC):
                    wo, wot = wstream(wproj[2, j])
                    bo, bot = nb()
                    for k in range(KC):
                        A("pe", lambda h, k=k, wo=wo, bo=bo: h.matmul(bo[:, :], lhsT=wo[:, k, :], rhs=mg[:, k, :], start=(k == 0), stop=(k == KC - 1)),
                          r=[wot, ("q", k)], w=[bot])
                    A("dve", lambda h, j=j, bo=bo: h.scalar_tensor_tensor(out=hT[:, j, :], in0=bo[:, :], scalar=0.5, in1=hT[:, j, :], op0=ALU.mult, op1=ALU.add),
                      r=[bot, ("h", j)], w=[("h", j)])
                for half in range(2):
                    A("sp", lambda h, half=half: h.dma_start(out=hmid[:, half * 8:(half + 1) * 8, ti * NT:(ti + 1) * NT],
                                                            in_=hT[:, half * 8:(half + 1) * 8, :]),
                      r=[("h", k) for k in range(half * 8, half * 8 + 8)], w=[("hmid", ti, half)], dma=True)

            def route_and_dispatch(ti):
                rmsnorm_to_xn(1, xn, "xn")
                for tb in range(4):
                    blk = ti * 4 + tb
                    b, bt = nb()
                    for k in range(KC):
                        A("pe", lambda h, k=k, tb=tb, b=b: h.matmul(b[:, 0:NE], lhsT=xn[:, k, tb * 128:(tb + 1) * 128], rhs=wrt[:, k, :],
                                                                    start=(k == 0), stop=(k == KC - 1)),
                          r=[("xn", k), "wrt"], w=[bt])
                    A("dve", lambda h, b=b: h.tensor_tensor(out=lg[:], in0=b[:, 0:NE], in1=cf[:, O_BROUT:O_BROUT + NE], op=ALU.add),
                      r=[bt, "cf"], w=["lg"])
                    A("dve", lambda h: h.max(out=top8[:], in_=lg[:]), r=["lg"], w=["top8"])
                    A("dve", lambda h: h.tensor_scalar(out=ntop[:], in0=top8[:, 0:1], scalar1=-1.0, scalar2=None, op0=ALU.mult),
                      r=["top8"], w=["ntop"])
                    A("dve", lambda h: h.tensor_scalar(out=mk[:], in0=lg[:], scalar1=top8[:, 3:4], scalar2=None, op0=ALU.is_ge),
                      r=["lg", "top8"], w=["mk"])
                    A("dve", lambda h: h.tensor_copy(out=mkb[:], in_=mk[:]), r=["mk"], w=["mkb"])
                    br_, brt = nb()
                    A("pe", lambda h, br_=br_: h.matmul(br_[:, 0:NE], lhsT=lt_bf, rhs=mkb[:], start=True, stop=True), r=["cbf", "mkb"], w=[brt])
                    A("pe", lambda h, br_=br_: h.matmul(br_[:, 64:64 + NE], lhsT=ones_bf, rhs=mkb[:], start=True, stop=True), r=["cbf", "mkb"], w=[brt])
                    A("dve", lambda h, br_=br_: h.tensor_tensor(out=rk[:], in0=br_[:, 0:NE], in1=cntb[:], op=ALU.add), r=[brt, "cntb"], w=["rk"])
                    A("dve", lambda h, br_=br_: h.tensor_tensor(out=cntb[:], in0=br_[:, 64:64 + NE], in1=cntb[:], op=ALU.add), r=[brt, "cntb", "rk"], w=["cntb"])
                    A("dve", lambda h: h.tensor_scalar(out=okr[:], in0=rk[:], scalar1=float(CAP), scalar2=None, op0=ALU.is_lt), r=["rk"], w=["okr"])
                    A("dve", lambda h: h.tensor_tensor(out=okr[:], in0=okr[:], in1=mk[:], op=ALU.mult), r=["okr", "mk"], w=["okr"])
                    A("dve", lambda h: h.tensor_tensor(out=rk[:], in0=rk[:], in1=cf[:, O_ECAP:O_ECAP + NE], op=ALU.add), r=["rk", "cf"], w=["rk"])
                    A("dve", lambda h: h.tensor_tensor(out=rk[:], in0=rk[:], in1=okr[:], op=ALU.mult), r=["rk", "okr"], w=["rk"])
                    A("dve", lambda h: h.tensor_scalar(out=rk[:], in0=rk[:], scalar1=BIG, scalar2=None, op0=ALU.add), r=["rk"], w=["rk"])
                    for kk in range(4):
                        A("dve", lambda h, kk=kk, blk=blk: h.scalar_tensor_tensor(
                            out=junk[:], in0=lg[:], scalar=top8[:, kk:kk + 1], in1=rk[:], op0=ALU.is_equal, op1=ALU.mult,
                            accum_out=dest_f[:, blk * 4 + kk:blk * 4 + kk + 1]),
                          r=["lg", "top8", "rk", "junk"], w=["junk", ("dest_f", blk)])
                    A("dve", lambda h, blk=blk: h.tensor_copy(out=dest_i[:, blk * 4:blk * 4 + 4], in_=dest_f[:, blk * 4:blk * 4 + 4]),
                      r=[("dest_f", blk)], w=[("dest_i", blk)])
                    A("act", lambda h: h.activation(out=ex4[:], in_=top8[:, 0:4], func=AF.Exp, bias=ntop[:, 0:1], accum_out=s4[:]),
                      r=["top8", "ntop"], w=["ex4", "s4"])
                    A("dve", lambda h: h.reciprocal(out=s4[:], in_=s4[:]), r=["s4"], w=["s4"])
                    A("dve", lambda h, blk=blk: h.tensor_scalar(out=w4[:, blk * 4:blk * 4 + 4], in0=ex4[:], scalar1=s4[:, 0:1], scalar2=None, op0=ALU.mult),
                      r=["ex4", "s4"], w=[("w4", blk)])
                    A("dve", lambda h, blk=blk: h.tensor_scalar(out=ex4[:], in0=dest_f[:, blk * 4:blk * 4 + 4], scalar1=BIG, scalar2=None, op0=ALU.is_lt),
                      r=[("dest_f", blk), "ex4"], w=["ex4"])
                    A("dve", lambda h, blk=blk: h.tensor_tensor(out=w4[:, blk * 4:blk * 4 + 4], in0=w4[:, blk * 4:blk * 4 + 4], in1=ex4[:], op=ALU.mult),
                      r=["ex4", ("w4", blk)], w=[("w4", blk)])
                    for half in range(2):
                        bT, bTt = nb()
                        bv_ = bT[:].bitcast(BF16)
                        for kq in range(8):
                            k = half * 8 + kq
                            A("pe", lambda h, k=k, kq=kq, tb=tb, bv_=bv_: h.transpose(out=bv_[:, kq * 128:(kq + 1) * 128],
                                                                                      in_=xn[:, k, tb * 128:(tb + 1) * 128], identity=ident_bf),
                              r=[("xn", k), "cbf"], w=[bTt])
                        A("act", lambda h, half=half, bv_=bv_: h.activation(out=xtok[:, half * 1024:(half + 1) * 1024], in_=bv_[:, 0:1024], func=AF.Copy),
                          r=[bTt], w=["xtok"])
                    for kk in range(4):
                        A("pool", lambda h, kk=kk, blk=blk: h.indirect_dma_start(
                            out=xg, out_offset=bass.IndirectOffsetOnAxis(ap=dest_i[:, blk * 4 + kk:blk * 4 + kk + 1], axis=0),
                            in_=xtok[:, :], in_offset=None, bounds_check=P.bc_reg, oob_is_err=False),
                          r=["xtok", ("dest_i", blk)], w=["xg"], dma=True)

            def load_tile(tok0):
                for half in range(2):
                    A("sp", lambda h, half=half: h.dma_start(out=hT[:, half * 8:(half + 1) * 8, :],
                                                            in_=xT[:, half * 8:(half + 1) * 8, tok0:tok0 + NT]),
                      w=[("h", k) for k in range(half * 8, half * 8 + 8)], dma=True)

            for pt in range(NTILE - n_prefix, NTILE):
                load_tile(pt * NT)
                rmsnorm_to_xn(0, xn, "xn")
                if pt == NTILE - 1:
                    kv_chunks()
                    kv_halo()
                for n in range(8):
                    for _ in lru_block(n, True, pt == 0, False):
                        pass
            A("dve", lambda h: h.tensor_scalar(out=hstate[:], in0=hstate[:], scalar1=flags[:, 0:1], scalar2=None, op0=ALU.mult),
              r=["hstate", "flags"], w=["hstate"])
            A("dve", lambda h: h.tensor_scalar(out=uh[:], in0=uh[:], scalar1=flags[:, 0:1], scalar2=None, op0=ALU.mult),
              r=["uh", "flags"], w=["uh"])
            for ti in range(n_own):
                load_tile(TOWN + ti * NT)
                if STAGE < 1:
                    continue
                rmsnorm_to_xn(0, xn, "xn")
                if STAGE < 2:
                    continue
                for c in range(KC):
                    b, bt = win_chunk(C_Q + c)
                    A("act", lambda h, b=b, c=c: h.activation(out=qT[:, c, :], in_=b[:, :], func=AF.Identity, bias=bias_col(C_Q + c)),
                      r=[bt, "cf"], w=[("q", c)])
                if STAGE < 3:
                    continue
                kv_chunks()
                if STAGE < 4:
                    continue
                def lru_all(ti=ti):
                    for n in range(8):
                        yield from lru_block(n, False, False, ti == 0)
                gens = [attention(ti == 0), lru_all()]
                while gens:
                    for g in list(gens):
                        try:
                            next(g)
                        except StopIteration:
                            gens.remove(g)
                if debug and ti == 0:
                    for nm, tl, rr in (("dbg_yat", yat, [("yat", k) for k in range(KC)]), ("dbg_q", qT, [("q", k) for k in range(KC)]),
                                       ("dbg_k", kT, ["kT"]), ("dbg_vlo", vlo, ["vlo"]), ("dbg_vhi", vhi, ["vhi"])):
                        dd = nc.dram_tensor(nm, list(tl.shape), BF16, kind="ExternalOutput").ap()
                        A("sp", lambda h, dd=dd, tl=tl: h.dma_start(out=dd, in_=tl[:]), r=rr, w=[nm], dma=True)
                kv_halo()
                if STAGE < 6:
                    continue
                merge_and_out(ti)
                if STAGE < 7:
                    continue
                route_and_dispatch(ti)
            barrier()
            P.flush()

        with contextlib.ExitStack() as st:
            def sb(name, shape, dt):
                return st.enter_context(nc.sbuf_tensor(name, list(shape), dt))

            xte = [sb("xte%d" % i, [128, NJT, D], BF16) for i in range(2)]
            xgT = [sb("xgT%d" % i, [128, KC, CAP], BF16) for i in range(2)]
            actT = [sb("actT%d" % i, [128, KC, CAP], BF16) for i in range(2)]
            w2b = [sb("w2b%d" % i, [128, KC, 512], BF16) for i in range(2)]
            ytok = [sb("ytok%d" % i, [128, NJT, D], F32) for i in range(2)]
            b2r = [sb("b2r%d" % i, [1, D], BF16) for i in range(2)]
            glt = [sb("glt%d" % i, [128, CAP], F32) for i in range(2)]
            sgt = [sb("sgt%d" % i, [128, CAP], F32) for i in range(2)]
            lnt = [sb("lnt%d" % i, [128, CAP], F32) for i in range(2)]
            w2rr = [0]
            for i in range(4, 8):
                wbufs.append(sb("wbufB%d" % i, [128, KC, 128], BF16))
            w2pend = {}

            def issue_w2(e, dg):
                ws = w2rr[0] % 2
                w2rr[0] += 1
                w2t = w2b[ws]
                A("pool", lambda h, w2t=w2t, e=e, dg=dg: h.dma_start(out=w2t[:], in_=w2_d[e, dg], max_dma_last_dim=4096),
                  w=[("w2b", ws)], dma=True)
                w2pend[(e, dg)] = (w2t, ws)

            for e in range(n_exp):
                s = e % 2
                xt_ = xte[s]
                for jt in range(NJT):
                    A("sp", lambda h, jt=jt, xt_=xt_, e=e: h.dma_start(out=xt_[0:JR[jt], jt, :], in_=xg[e * CAP + jt * 128:e * CAP + jt * 128 + JR[jt], :]),
                      r=["xg"], w=[("xte", s, jt)], dma=True)
                A("pool", lambda h, e=e, s=s: h.dma_start(out=b2r[s][:], in_=b2_d[e:e + 1, :]), w=[("b2r", s)], dma=True)
                xg_ = xgT[s]
                for jt in range(NJT):
                    for half in range(2):
                        bT, bTt = nb()
                        bv_ = bT[:].bitcast(BF16)
                        for kq in range(8):
                            k = half * 8 + kq
                            A("pe", lambda h, k=k, kq=kq, jt=jt, bv_=bv_, xt_=xt_: h.transpose(
                                out=bv_[:, kq * 128:kq * 128 + JR[jt]], in_=xt_[0:JR[jt], jt, k * 128:(k + 1) * 128],
                                identity=ident_bf[0:JR[jt], 0:JR[jt]]),
                              r=[("xte", s, jt), "cbf"], w=[bTt])
                        A("act", lambda h, half=half, jt=jt, bv_=bv_, xg_=xg_: h.activation(
                            out=xg_[:, half * 8:(half + 1) * 8, jt * 128:jt * 128 + JR[jt]],
                            in_=bv_[:, 0:1024].rearrange("p (a c) -> p a c", c=128)[:, :, 0:JR[jt]], func=AF.Copy),
                          r=[bTt], w=[("xgT", s)])
                at_ = actT[s]
                for i in range(KC):
                    bks = []
                    for fc in (i, KC + i):
                        wt, wtok = wstream(w1_d[e, fc])
                        b, bt = nb()
                        for k in range(KC):
                            A("pe", lambda h, k=k, wt=wt, b=b, xg_=xg_: h.matmul(b[:, 0:CAP], lhsT=wt[:, k, :], rhs=xg_[:, k, :],
                                                                                start=(k == 0), stop=(k == KC - 1)),
                              r=[wtok, ("xgT", s)], w=[bt])
                        bks.append((b, bt))
                    (bg, bgt), (bl, blt) = bks
                    if i == 9:
                        issue_w2(e, 0)
                    if i == 12:
                        issue_w2(e, 1)
                    g_, s_, l_ = glt[i % 2], sgt[i % 2], lnt[i % 2]
                    cg = O_B1 + e * 32 + i
                    cl = O_B1 + e * 32 + KC + i
                    A("dve", lambda h, bg=bg, g_=g_, cg=cg: h.tensor_scalar(out=g_[:], in0=bg[:, 0:CAP], scalar1=cf[:, cg:cg + 1], scalar2=7.0,
                                                                            op0=ALU.add, op1=ALU.min),
                      r=[bgt, "cf"], w=[("glt", i % 2)])
                    A("act", lambda h, g_=g_, s_=s_: h.activation(out=s_[:], in_=g_[:], func=AF.Sigmoid, scale=1.702),
                      r=[("glt", i % 2)], w=[("sgt", i % 2)])
                    A("dve", lambda h, bl=bl, l_=l_, cl=cl: h.tensor_scalar(out=l_[:], in0=bl[:, 0:CAP], scalar1=cf[:, cl:cl + 1], scalar2=7.0,
                                                                            op0=ALU.add, op1=ALU.min),
                      r=[blt, "cf"], w=[("lnt", i % 2)])
                    A("dve", lambda h, l_=l_: h.tensor_scalar(out=l_[:], in0=l_[:], scalar1=-7.0, scalar2=1.0, op0=ALU.max, op1=ALU.add),
                      r=[("lnt", i % 2)], w=[("lnt", i % 2)])
                    A("dve", lambda h, g_=g_, s_=s_: h.tensor_tensor(out=g_[:], in0=g_[:], in1=s_[:], op=ALU.mult),
                      r=[("glt", i % 2), ("sgt", i % 2)], w=[("glt", i % 2)])
                    A("dve", lambda h, g_=g_, l_=l_, i=i, at_=at_: h.tensor_tensor(out=at_[:, i, :], in0=g_[:], in1=l_[:], op=ALU.mult),
                      r=[("glt", i % 2), ("lnt", i % 2)], w=[("actT", s, i)])
                yt_ = ytok[s]
                for dg in range(4):
                    w2t, ws = w2pend.pop((e, dg))
                    for jt in range(NJT):
                        b, bt = nb()
                        for k in range(KC):
                            A("pe", lambda h, k=k, jt=jt, b=b, w2t=w2t, at_=at_: h.matmul(
                                b[0:JR[jt], :], lhsT=at_[:, k, jt * 128:jt * 128 + JR[jt]], rhs=w2t[:, k, :], start=(k == 0), stop=False),
                              r=[("actT", s, k), ("w2b", ws)], w=[bt])
                        A("pe", lambda h, b=b, dg=dg, s=s, jt=jt: h.matmul(b[0:JR[jt], :], lhsT=ones_bf[0:1, 0:JR[jt]],
                                                                           rhs=b2r[s][0:1, dg * 512:(dg + 1) * 512], start=False, stop=True),
                          r=["cbf", ("b2r", s)], w=[bt])
                        A("act", lambda h, b=b, jt=jt, dg=dg, yt_=yt_: h.activation(out=yt_[0:JR[jt], jt, dg * 512:(dg + 1) * 512], in_=b[0:JR[jt], :],
                                                                                    func=AF.Copy),
                          r=[bt], w=[("ytok", s, jt)])
                    if dg < 2:
                        issue_w2(e, dg + 2)
                for jt in range(NJT):
                    A("sp", lambda h, jt=jt, yt_=yt_, e=e: h.dma_start(out=yg[e * CAP + jt * 128:e * CAP + jt * 128 + JR[jt], :], in_=yt_[0:JR[jt], jt, :]),
                      r=[("ytok", s, jt)], w=["yg"], dma=True)
            del wbufs[4:]
            barrier()
            P.flush()

        with contextlib.ExitStack() as st:
            def sb(name, shape, dt):
                return st.enter_context(nc.sbuf_tensor(name, list(shape), dt))

            hT = sb("hT_c", [128, KC, NT], F32)
            xn = sb("xn_c", [128, KC, NT], BF16)
            gbs = [[sb("gb%d_%d" % (p_, i), [128, D], F32) for i in range(4)] for p_ in range(2)]
            accs = [sb("acc%d" % p_, [128, D], F32) for p_ in range(2)]
            wple = sb("wple_t", [128, 2, D], BF16)
            ptb = sb("ptb", [128, 2, NT], BF16)
            sqb = [sb("sqb_c%d" % i, [128, NT], BF16) for i in range(2)]
            rs = sb("rs_c", [128, NT], F32)
            rstd = sb("rstd_c", [128, NT], F32)
            gate = [sb("gate%d" % i, [128, NT], F32) for i in range(2)]
            tmp = [sb("tmp%d" % i, [128, NT], F32) for i in range(2)]

            A("pool", lambda h: h.dma_start(out=wple[:], in_=wple_d, max_dma_last_dim=4096), w=["wple"], dma=True)
            for p_ in range(2):
                for kk in range(4):
                    A("dve", lambda h, kk=kk, p_=p_: h.memset(gbs[p_][kk][:], 0.0), w=[("gb", p_, kk)])

            def norm_c(gidx, dst_fn, dst_tok):
                b, bt = nb()
                for k in range(KC):
                    sq = sqb[k % 2]
                    A("act", lambda h, k=k, sq=sq: h.activation(out=sq[:], in_=hT[:, k, :], func=AF.Square),
                      r=[("hc", k)], w=[("sqc", k % 2)])
                    A("pe", lambda h, k=k, sq=sq: h.matmul(b[:, :], lhsT=ones_bf, rhs=sq[:], start=(k == 0), stop=(k == KC - 1)),
                      r=[("sqc", k % 2), "cbf"], w=[bt])
                A("act", lambda h: h.activation(out=rs[:], in_=b[:, :], func=AF.Sqrt, scale=1.0 / D, bias=EPS), r=[bt], w=["rsc"])
                A("dve", lambda h: h.reciprocal(out=rstd[:], in_=rs[:]), r=["rsc"], w=["rstdc"])
                for k in range(KC):
                    A("dve", lambda h, k=k: h.scalar_tensor_tensor(
                        out=dst_fn(k), in0=hT[:, k, :], scalar=cf[:, O_GAIN + gidx * 16 + k:O_GAIN + gidx * 16 + k + 1],
                        in1=rstd[:], op0=ALU.mult, op1=ALU.mult),
                      r=[("hc", k), "rstdc", "cf"], w=[(dst_tok, k)])

            for ti in range(n_own if final else 0):
                for half in range(2):
                    A("sp", lambda h, half=half, ti=ti: h.dma_start(out=hT[:, half * 8:(half + 1) * 8, :],
                                                                    in_=hmid[:, half * 8:(half + 1) * 8, ti * NT:(ti + 1) * NT]),
                      r=[("hmid", ti, half)], w=[("hc", k) for k in range(half * 8, half * 8 + 8)], dma=True)
                A("pool", lambda h, ti=ti: h.dma_start(out=ptb[:], in_=pT[:, :, ti * NT:(ti + 1) * NT]), w=["ptb"], dma=True)
                for tb in range(4):
                    blk = ti * 4 + tb
                    pp = blk % 2
                    gb = gbs[pp]
                    acc = accs[pp]
                    for kk in range(4):
                        A("pool", lambda h, kk=kk, blk=blk, gb=gb: h.indirect_dma_start(
                            out=gb[kk][:, :], out_offset=None, in_=yg,
                            in_offset=bass.IndirectOffsetOnAxis(ap=dest_i[:, blk * 4 + kk:blk * 4 + kk + 1], axis=0),
                            bounds_check=P.bc_reg, oob_is_err=False),
                          r=["yg", ("dest_i", blk)], w=[("gb", pp, kk)], dma=True)
                    A("dve", lambda h, blk=blk, gb=gb, acc=acc: h.tensor_scalar(out=acc[:], in0=gb[0][:], scalar1=w4[:, blk * 4:blk * 4 + 1], scalar2=None, op0=ALU.mult),
                      r=[("gb", pp, 0), ("w4", blk)], w=[("acc", pp)])
                    for kk in range(1, 4):
                        A("dve", lambda h, kk=kk, blk=blk, gb=gb, acc=acc: h.scalar_tensor_tensor(
                            out=acc[:], in0=gb[kk][:], scalar=w4[:, blk * 4 + kk:blk * 4 + kk + 1], in1=acc[:], op0=ALU.mult, op1=ALU.add),
                          r=[("gb", pp, kk), ("w4", blk), ("acc", pp)], w=[("acc", pp)])
                    for q4 in range(4):
                        b, bt = nb()
                        for kq in range(4):
                            k = q4 * 4 + kq
                            A("pe", lambda h, k=k, kq=kq, b=b, acc=acc: h.transpose(out=b[:, kq * 128:(kq + 1) * 128], in_=acc[:, k * 128:(k + 1) * 128],
                                                                                    identity=ident_f),
                              r=[("acc", pp), "cf"], w=[bt])
                        A("dve", lambda h, q4=q4, tb=tb, b=b: h.tensor_tensor(
                            out=hT[:, q4 * 4:(q4 + 1) * 4, tb * 128:(tb + 1) * 128], in0=b[:, :].rearrange("p (a c) -> p a c", c=128),
                            in1=hT[:, q4 * 4:(q4 + 1) * 4, tb * 128:(tb + 1) * 128], op=ALU.add),
                          r=[bt] + [("hc", k) for k in range(q4 * 4, q4 * 4 + 4)], w=[("hc", k) for k in range(q4 * 4, q4 * 4 + 4)])
                norm_c(2, lambda k: xn[:, k, :], "xnc")
                for j in range(KC):
                    wg_, wgt = wstream(wproj[3, j])
                    bg, bgt = nb()
                    for k in range(KC):
                        A("pe", lambda h, k=k, wg_=wg_, bg=bg: h.matmul(bg[:, :], lhsT=wg_[:, k, :], rhs=xn[:, k, :], start=(k == 0), stop=(k == KC - 1)),
                          r=[wgt, ("xnc", k)], w=[bgt])
                    bp, bpt = nb()
                    for k in range(2):
                        A("pe", lambda h, k=k, j=j, bp=bp: h.matmul(bp[:, :], lhsT=wple[:, k, j * 128:(j + 1) * 128], rhs=ptb[:, k, :],
                                                                    start=(k == 0), stop=(k == 1)),
                          r=["wple", "ptb"], w=[bpt])
                    g_ = gate[j % 2]
                    t_ = tmp[j % 2]
                    A("act", lambda h, bg=bg, g_=g_: h.activation(out=g_[:], in_=bg[:, :], func=AF.Sigmoid), r=[bgt], w=[("gate", j % 2)])
                    A("dve", lambda h, bp=bp, g_=g_, t_=t_: h.tensor_tensor(out=t_[:], in0=bp[:, :], in1=g_[:], op=ALU.mult),
                      r=[bpt, ("gate", j % 2)], w=[("tmp", j % 2)])
                    A("dve", lambda h, j=j, t_=t_: h.tensor_tensor(out=hT[:, j, :], in0=hT[:, j, :], in1=t_[:], op=ALU.add),
                      r=[("tmp", j % 2), ("hc", j)], w=[("hc", j)])
                norm_c(3, lambda k: hT[:, k, :], "hc")
                for half in range(2):
                    A("sp", lambda h, half=half, ti=ti: h.dma_start(out=outT[:, half * 8:(half + 1) * 8, ti * NT:(ti + 1) * NT],
                                                                    in_=hT[:, half * 8:(half + 1) * 8, :]),
                      r=[("hc", k) for k in range(half * 8, half * 8 + 8)], w=[("out", ti, half)], dma=True)
            P.flush(final=True)
    return nc


def _chunk_w(w):
    K, M = w.shape
    return np.ascontiguousarray(w.reshape(KC, 128, M // 128, 128).transpose(2, 1, 0, 3))


def _pcol(v):
    return np.ascontiguousarray(v.reshape(-1, 128).T)


def _host_layout(inp):
    f = np.float32
    w_in = inp["w_in"][0]
    b_in = inp["b_in"][0]
    QW, KVW = 2048, 256
    oq, ok, ov = 0, QW, QW + KVW
    ou = QW + 2 * KVW
    og = ou + 2048
    oga = og + 2048
    ogl = oga + 2048
    cols = []
    for c in range(16):
        cols.append(np.arange(oq + c * 128, oq + (c + 1) * 128))
    for kvh in range(4):
        a = np.arange(ok + kvh * 64, ok + (kvh + 1) * 64)
        cols.append(np.concatenate([a, a]))
    for n in range(8):
        cols.append(np.arange(ou + (2 * n) * 128, ou + (2 * n + 1) * 128))
        cols.append(np.arange(ou + (2 * n + 1) * 128, ou + (2 * n + 2) * 128))
        cols.append(np.arange(og + (2 * n) * 128, og + (2 * n + 1) * 128))
        cols.append(np.arange(og + (2 * n + 1) * 128, og + (2 * n + 2) * 128))
    for j in range(16):
        cols.append(np.arange(oga + j * 128, oga + (j + 1) * 128))
        cols.append(np.arange(ogl + j * 128, ogl + (j + 1) * 128))
    cols = np.concatenate(cols)
    assert cols.size == NWIN * 128
    win = _chunk_w(w_in[:, cols])
    bin_ = _pcol(b_in[cols])
    wv = np.ascontiguousarray(w_in[:, ov:ov + 256].reshape(KC, 128, 256).transpose(1, 0, 2))
    bv = np.broadcast_to(b_in[ov:ov + 256][None, :], (128, 256))
    wproj = np.stack([_chunk_w(inp["w_attn_proj"][0]), _chunk_w(inp["w_lru_proj"][0]),
                      _chunk_w(inp["w_out"][0]), _chunk_w(inp["w_ple_gate"][0])])
    wa = inp["w_rg_a"][0].reshape(8, 2, 128, 256).transpose(0, 2, 1, 3)
    wx = inp["w_rg_x"][0].reshape(8, 2, 128, 256).transpose(0, 2, 1, 3)
    wrg = np.ascontiguousarray(np.stack([wa, wx], axis=2))
    brg = np.concatenate([_pcol(inp["b_rg_a"][0].reshape(-1)), _pcol(inp["b_rg_x"][0].reshape(-1))], axis=1)
    convw = np.concatenate([_pcol(inp["conv_w"][0][t]) for t in range(4)], axis=1)
    convb = _pcol(inp["conv_b"][0])
    lam = _pcol(inp["lru_lambda"][0])
    gains = np.concatenate([_pcol(inp["norm_mix_g"][0]), _pcol(inp["norm_ffn_g"][0]),
                            _pcol(inp["norm_ple_g"][0]), _pcol(inp["norm_final_g"])], axis=1)
    sink = _pcol(np.repeat(inp["attn_sinks"][0], 64))
    wrouter = np.ascontiguousarray(inp["w_router"][0].reshape(KC, 128, NE).transpose(1, 0, 2))
    brout = np.broadcast_to(inp["b_router"][0][None, :], (128, NE))
    wple = np.ascontiguousarray(inp["w_ple"][0].reshape(2, 128, D).transpose(1, 0, 2))
    w1 = inp["w_mlp1"][0]
    w1l = np.ascontiguousarray(w1.reshape(NE, KC, 128, 32, 128).transpose(0, 3, 2, 1, 4))
    b1 = inp["b_mlp1"][0].reshape(NE, 32, 128).transpose(2, 0, 1).reshape(128, NE * 32)
    w2 = inp["w_mlp2"][0]
    w2l = np.ascontiguousarray(w2.reshape(NE, KC, 128, 4, 512).transpose(0, 3, 2, 1, 4))
    b2 = np.ascontiguousarray(inp["b_mlp2"][0])
    ecap = np.broadcast_to((np.arange(NE, dtype=f) * CAP - BIG)[None, :], (128, NE))
    cf = np.zeros((128, NCF), f)
    cf[:, O_IDF:O_IDF + 128] = np.eye(128, dtype=f)
    cf[:, O_ECAP:O_ECAP + NE] = ecap
    cf[:, O_BROUT:O_BROUT + NE] = brout
    cf[:, O_BV:O_BV + 256] = bv
    cf[:, O_BIN:O_BIN + NWIN] = bin_
    cf[:, O_BRG:O_BRG + 32] = brg
    cf[:, O_CONVW:O_CONVW + 64] = convw
    cf[:, O_CONVB:O_CONVB + 16] = convb
    cf[:, O_LAM:O_LAM + 16] = lam
    cf[:, O_GAIN:O_GAIN + 64] = gains
    cf[:, O_SINK:O_SINK + 16] = sink
    cf[:, O_B1:O_B1 + 1024] = b1
    NEG = -30000.0
    jj = np.arange(128)[:, None]
    ii = np.arange(128)[None, :]
    mbprev = np.where(jj > ii, 0.0, NEG).astype(f)
    mbcur = np.where(jj <= ii, 0.0, NEG).astype(f)
    cbf = np.zeros((128, NCB), f)
    cbf[:, B_ID:B_ID + 128] = np.eye(128, dtype=f)
    cbf[:, B_LT:B_LT + 128] = (jj < ii).astype(f)
    cbf[:, B_ONES:B_ONES + 128] = 1.0
    cbf[:, B_MB4:B_MB4 + 512] = np.concatenate([mbprev, mbcur, mbprev, mbcur], axis=1)
    cbf[:, B_OLO:B_OLO + 64] = 1.0
    cbf[:, B_OHI + 64:B_OHI + 128] = 1.0
    shared = dict(cbf=cbf, cf=cf, win=win, wv=wv, wproj=wproj, wrg=wrg, wrouter=wrouter, wple=wple,
                  w1=w1l, w2=w2l, b2=b2)
    x = inp["x"]
    p = inp["p"][0]
    in_maps = []
    for core in range(NCORES):
        b, half = core // 2, core % 2
        xt = x[b].T.reshape(KC, 128, 2 * TOWN).transpose(1, 0, 2)
        if half == 0:
            xT = np.concatenate([np.zeros((128, KC, TOWN), f), xt[:, :, :TOWN]], axis=2)
        else:
            xT = xt
        pt = p[b, half * TOWN:(half + 1) * TOWN].T.reshape(2, 128, TOWN).transpose(1, 0, 2)
        ccv = np.zeros((128, 516), f)
        mb0 = np.full((128, 128), NEG, f) if half == 0 else mbprev
        ccv[:, 0:512] = np.concatenate([mb0, mbcur, mb0, mbcur], axis=1)
        ccv[:, 512] = float(half)
        ccv[:, 513] = float(half)
        ccv[:, 514] = float(1 - half)
        m = dict(shared)
        m["xT"] = np.ascontiguousarray(xT, dtype=f)
        m["pT"] = np.ascontiguousarray(pt, dtype=f)
        m["cc"] = ccv
        in_maps.append(m)
    return in_maps


_NC_CACHE = {}


def kernel(**inputs):
    inp = {k: np.asarray(v) for k, v in inputs.items()}
    in_maps = _host_layout(inp)
    if "nc" not in _NC_CACHE:
        _NC_CACHE["nc"] = build_nc()
    nc = _NC_CACHE["nc"]
    res = run_bass_kernel_spmd(nc, in_maps, core_ids=list(range(NCORES)))
    out = np.empty((4, 4096, D), np.float32)
    for core in range(NCORES):
        b, half = core // 2, core % 2
        o = res.results[core]["outT"]
        out[b, half * TOWN:(half + 1) * TOWN, :] = o.transpose(2, 1, 0).reshape(TOWN, D)
    return out
```

```python
import contextlib
import numpy as np
import concourse.bass as bass
import concourse.mybir as mybir
from concourse.bass_utils import run_bass_kernel_spmd

F32 = mybir.dt.float32
BF16 = mybir.dt.bfloat16
I32 = mybir.dt.int32
ALU = mybir.AluOpType
AF = mybir.ActivationFunctionType

D = 2048
KC = 16
NT = 512
TOWN = 2048
NTILE = TOWN // NT
NE = 32
CAP = 352
NJT = 3
JR = (128, 128, 96)
BIG = 65536.0
EPS = 1e-6
NCORES = 8
STAGE = 99

C_Q = 0
C_K = 16
C_LRU = 20
C_GATE = 52
NWIN = 84

O_IDF = 0
O_ECAP = 128
O_BROUT = 160
O_BV = 192
O_BIN = 448
O_BRG = O_BIN + NWIN
O_CONVW = O_BRG + 32
O_CONVB = O_CONVW + 64
O_LAM = O_CONVB + 16
O_GAIN = O_LAM + 16
O_SINK = O_GAIN + 64
O_B1 = O_SINK + 16
NCF = O_B1 + 1024
B_ID = 0
B_LT = 128
B_ONES = 256
B_MB4 = 384
B_OLO = 896
B_OHI = 1024
NCB = 1152

ENGS = ("pe", "act", "dve", "pool", "sp")


class Op:
    __slots__ = ("eng", "fn", "deps", "is_dma", "sem", "cnt", "signal", "sig_idx")


class Prog:
    def __init__(self, nc, n_dma_sems=8):
        self.nc = nc
        self.ops = {e: [] for e in ENGS}
        self.last_w = {}
        self.readers = {}
        self.n_dma_sems = n_dma_sems
        self.dma_rr = {e: 0 for e in ENGS}
        self.dma_last = {}
        self.dma_cnt = {}

    def add(self, eng, fn, r=(), w=(), dma=False, extra=()):
        op = Op()
        op.eng = eng; op.fn = fn; op.is_dma = dma; op.signal = False; op.sig_idx = 0
        op.sem = None; op.cnt = 0
        deps = list(extra)
        for t in r:
            x = self.last_w.get(t)
            if x is not None:
                deps.append(x)
        for t in w:
            x = self.last_w.get(t)
            if x is not None:
                deps.append(x)
            deps.extend(self.readers.get(t, ()))
        if dma:
            slot = self.dma_rr[eng] % self.n_dma_sems
            self.dma_rr[eng] += 1
            key = (eng, slot)
            prev = self.dma_last.get(key)
            if prev is not None:
                deps.append(prev)
            self.dma_last[key] = op
            self.dma_cnt[key] = self.dma_cnt.get(key, 0) + 16
            op.sem = key
            op.cnt = self.dma_cnt[key]
        op.deps = deps
        for t in r:
            self.readers.setdefault(t, []).append(op)
        for t in w:
            self.last_w[t] = op
            self.readers[t] = []
        self.ops[eng].append(op)
        return op

    def all_tails(self):
        tails = []
        for e in ENGS:
            for op in reversed(self.ops[e]):
                if not op.is_dma:
                    tails.append(op)
                    break
        tails.extend(self.dma_last.values())
        return tails

    def setup(self, st):
        nc = self.nc
        self.esem = {e: st.enter_context(nc.semaphore("s_" + e)) for e in ENGS}
        self.dsem = {}
        for e in ("sp", "pool", "act"):
            for i in range(self.n_dma_sems):
                self.dsem[(e, i)] = st.enter_context(nc.semaphore("d_%s_%d" % (e, i)))
        self.emitted = {e: 0 for e in ENGS}
        self.sigc = {e: 0 for e in ENGS}
        self.waited_e = {e: {x: 0 for x in ENGS} for e in ENGS}
        self.waited_d = {e: {} for e in ENGS}
        self.done_ops = set()

    def flush(self, final=False):
        nc = self.nc
        pend = {e: self.ops[e][self.emitted[e]:] for e in ENGS}
        for e in ENGS:
            for op in pend[e]:
                for d in op.deps:
                    if not d.is_dma and not (d.eng == e and e == "pe") and id(d) not in self.done_ops:
                        d.signal = True
        for e in ENGS:
            for op in pend[e]:
                if (not op.is_dma) and op.signal:
                    self.sigc[e] += 1
                    op.sig_idx = self.sigc[e]
        prog = self
        esem, dsem = self.esem, self.dsem
        with nc.Block() as block:
            def run(e, h):
                if e == "pool":
                    prog.bc_reg = h.to_reg(NE * CAP - 1)
                waited_e = prog.waited_e[e]
                waited_d = prog.waited_d[e]
                for op in pend[e]:
                    need_e = {}
                    need_d = {}
                    for d in op.deps:
                        if d.is_dma:
                            if waited_d.get(d.sem, 0) < d.cnt:
                                need_d[d.sem] = max(need_d.get(d.sem, 0), d.cnt)
                        else:
                            if d.eng == e and e == "pe":
                                continue
                            if d.sig_idx == 0:
                                assert id(d) in prog.done_ops, "unsignalled dep in same phase"
                                continue
                            if waited_e[d.eng] < d.sig_idx:
                                need_e[d.eng] = max(need_e.get(d.eng, 0), d.sig_idx)
                    for x, v in need_e.items():
                        h.wait_ge(esem[x], v); waited_e[x] = v
                    for k, v in need_d.items():
                        h.wait_ge(dsem[k], v); waited_d[k] = v
                    ins = op.fn(h)
                    if op.is_dma:
                        ins.then_inc(dsem[op.sem], 16)
                    elif op.signal:
                        ins.then_inc(esem[e], 1)
                if final:
                    for key, cnt in prog.dma_cnt.items():
                        if key[0] == e:
                            h.wait_ge(dsem[key], cnt)

            @block.sync
            def _(h):
                run("sp", h)

            @block.scalar
            def _(h):
                run("act", h)

            @block.vector
            def _(h):
                run("dve", h)

            @block.gpsimd
            def _(h):
                run("pool", h)

            @block.tensor
            def _(h):
                run("pe", h)
        for e in ENGS:
            for op in pend[e]:
                self.done_ops.add(id(op))
            self.emitted[e] = len(self.ops[e])


def build_nc(debug=False, n_prefix=NTILE, n_own=NTILE, n_exp=NE, final=True):
    NEd = max(1, n_exp)
    nc = bass.Bass("TRN2", target_bir_lowering=False)
    P = Prog(nc)
    A = P.add

    def din(name, shape, dt=F32):
        return nc.dram_tensor(name, list(shape), dt, kind="ExternalInput").ap()

    xT = din("xT", [128, KC, 2 * TOWN])
    pT = din("pT", [128, 2, TOWN])
    cc = din("cc", [128, 516])
    cbf_d = din("cbf", [128, NCB])
    cf_d = din("cf", [128, NCF])
    win = din("win", [NWIN, 128, KC, 128])
    wv_d = din("wv", [128, KC, 256])
    wproj = din("wproj", [4, KC, 128, KC, 128])
    wrg_d = din("wrg", [8, 128, 2, 2, 256])
    wrouter_d = din("wrouter", [128, KC, NE])
    wple_d = din("wple", [128, 2, D])
    w1_d = din("w1", [NEd, 32, 128, KC, 128])
    w2_d = din("w2", [NEd, 4, 128, KC, 512])
    b2_d = din("b2", [NEd, D])
    outT = nc.dram_tensor("outT", [128, KC, TOWN], F32, kind="ExternalOutput").ap()
    skind = "ExternalOutput" if debug else "Internal"
    hmid = nc.dram_tensor("hmid", [128, KC, TOWN], F32, kind=skind).ap()
    xg = nc.dram_tensor("xg", [NE * CAP, D], BF16, kind="Internal").ap()
    yg = nc.dram_tensor("yg", [NE * CAP, D], F32, kind="Internal").ap()

    with contextlib.ExitStack() as gst:
        def sbg(name, shape, dt):
            return gst.enter_context(nc.sbuf_tensor(name, list(shape), dt))

        P.setup(gst)
        banks = [gst.enter_context(nc.psum_tensor("bank%d" % i, [128, 512], F32)) for i in range(8)]
        bank_rr = [0]

        def nb():
            i = bank_rr[0] % 8
            bank_rr[0] += 1
            return banks[i], ("bank", i)

        cbf = sbg("cbf_t", [128, NCB], BF16)
        cf = sbg("cf_t", [128, NCF], F32)
        mb40 = sbg("mb40", [128, 512], BF16)
        flags = sbg("flags", [128, 4], F32)
        wbufs = [sbg("wbuf%d" % i, [128, KC, 128], BF16) for i in range(4)]
        wrr = [0]
        dest_f = sbg("dest_f", [128, 64], F32)
        dest_i = sbg("dest_i", [128, 64], I32)
        w4 = sbg("w4", [128, 64], F32)
        bsc = sbg("bsc", [128, 8], F32)

        ident_bf = cbf[:, B_ID:B_ID + 128]
        lt_bf = cbf[:, B_LT:B_LT + 128]
        ones_bf = cbf[:, B_ONES:B_ONES + 128]
        mb4 = cbf[:, B_MB4:B_MB4 + 512]
        oneslo = cbf[:, B_OLO:B_OLO + 128]
        oneshi = cbf[:, B_OHI:B_OHI + 128]
        ident_f = cf[:, O_IDF:O_IDF + 128]

        A("pool", lambda h: h.dma_start(out=cbf[:], in_=cbf_d), w=["cbf"], dma=True)
        A("pool", lambda h: h.dma_start(out=mb40[:], in_=cc[:, 0:512]), w=["mb40"], dma=True)
        A("sp", lambda h: h.dma_start(out=cf[:], in_=cf_d), w=["cf"], dma=True)
        A("sp", lambda h: h.dma_start(out=flags[:], in_=cc[:, 512:516]), w=["flags"], dma=True)

        def wstream(src):
            i = wrr[0] % len(wbufs)
            wrr[0] += 1
            t = wbufs[i]
            A("pool", lambda h: h.dma_start(out=t[:], in_=src), w=[("wb", i)], dma=True)
            return t, ("wb", i)

        bar_n = [0]

        def barrier():
            tails = P.all_tails()
            n = bar_n[0]
            bar_n[0] += 1
            for e in ("act", "dve", "pool"):
                col = {"act": 0, "dve": 1, "pool": 2}[e]
                if e == "act":
                    A(e, lambda h, col=col: h.memzero(bsc[:, col:col + 1]), w=[("bsc", e)], extra=tails)
                else:
                    A(e, lambda h, col=col: h.memset(bsc[:, col:col + 1], 0.0), w=[("bsc", e)], extra=tails)
            b, bt = nb()
            A("pe", lambda h: h.matmul(b[0:1, 0:2], lhsT=ones_bf[0:1, 0:1], rhs=ones_bf[0:1, 0:2], start=True, stop=True),
              r=["cbf"], w=[bt], extra=tails)
            A("sp", lambda h: h.dma_start(out=bsc[:, 4:8], in_=cc[:, 512:516]), w=[("bsc", "sp")], dma=True, extra=tails)

        with contextlib.ExitStack() as st:
            def sb(name, shape, dt):
                return st.enter_context(nc.sbuf_tensor(name, list(shape), dt))

            hT = sb("hT", [128, KC, NT], F32)
            xn = sb("xn", [128, KC, NT], BF16)
            qT = sb("qT", [128, KC, NT], BF16)
            kT = sb("kT", [128, 4, NT + 128], BF16)
            vlo = sb("vlo", [128, 5, 4, 128], BF16)
            vhi = sb("vhi", [128, 5, 4, 128], BF16)
            yat = sb("yat", [128, KC, NT], BF16)
            ylr = sb("ylr", [128, KC, NT], BF16)
            wv = sb("wv_t", [128, KC, 256], BF16)
            wrgt = [sb("wrgt%d" % i, [128, 2, 2, 256], BF16) for i in range(2)]
            wrt = sb("wrt", [128, KC, NE], BF16)
            sqb = [sb("sqb%d" % i, [128, NT], BF16) for i in range(2)]
            rs = sb("rs", [128, NT], F32)
            rstd = sb("rstd", [128, NT], F32)
            u = sb("u", [128, 2, NT + 3], F32)
            uc = sb("uc", [128, 2, NT], F32)
            ucb = sb("ucb", [128, 2, NT], BF16)
            uh = sb("uh", [128, KC, 3], F32)
            hstate = sb("hstate", [128, KC], F32)
            rt = [sb("rt%d" % i, [128, NT], F32) for i in range(2)]
            igt = [sb("igt%d" % i, [128, NT], F32) for i in range(2)]
            at = [sb("at%d" % i, [128, NT], F32) for i in range(2)]
            a2t = [sb("a2t%d" % i, [128, NT], F32) for i in range(2)]
            hseq = [sb("hseq%d" % i, [128, NT], F32) for i in range(2)]
            gt = [sb("gt%d" % i, [128, NT], F32) for i in range(2)]
            t1 = [sb("t1%d" % i, [128, NT], F32) for i in range(2)]
            pm = [sb("pm%d" % i, [128, 512], BF16) for i in range(3)]
            dent = [sb("dent%d" % i, [128, 128], F32) for i in range(2)]
            rect = [sb("rect%d" % i, [128, 128], F32) for i in range(2)]
            nsp8 = sb("nsp8", [128, KC], F32)
            nsp16 = sb("nsp16", [128, KC], F32)
            esink = sb("esink", [128, KC], F32)
            hbrg = sb("hbrg", [128, 32], F32)
            hbin = sb("hbin", [128, NWIN], F32)
            xtok = sb("xtok", [128, D], BF16)
            lg = sb("lg", [128, NE], F32)
            top8 = sb("top8", [128, 8], F32)
            ntop = sb("ntop", [128, 1], F32)
            mk = sb("mk", [128, NE], F32)
            mkb = sb("mkb", [128, NE], BF16)
            rk = sb("rk", [128, NE], F32)
            okr = sb("okr", [128, NE], F32)
            cntb = sb("cntb", [128, NE], F32)
            ex4 = sb("ex4", [128, 4], F32)
            s4 = sb("s4", [128, 1], F32)
            junk = sb("junk", [128, NE], F32)

            A("pool", lambda h: h.dma_start(out=wv[:], in_=wv_d), w=["wv"], dma=True)
            A("pool", lambda h: h.dma_start(out=wrt[:], in_=wrouter_d), w=["wrt"], dma=True)
            A("dve", lambda h: h.memset(vlo[:], 0.0), w=["vlo"])
            A("dve", lambda h: h.memset(vhi[:], 0.0), w=["vhi"])
            A("dve", lambda h: h.memset(kT[:], 0.0), w=["kT"])
            A("dve", lambda h: h.memset(uh[:], 0.0), w=["uh"])
            A("dve", lambda h: h.memset(hstate[:], 0.0), w=["hstate"])
            A("dve", lambda h: h.memset(cntb[:], 0.0), w=["cntb"])
            A("act", lambda h: h.activation(out=nsp8[:], in_=cf[:, O_LAM:O_LAM + 16], func=AF.Exp, scale=-1.0),
              r=["cf"], w=["nsp8"])
            A("act", lambda h: h.activation(out=nsp8[:], in_=nsp8[:], func=AF.Ln, bias=1.0), r=["nsp8"], w=["nsp8"])
            A("dve", lambda h: h.tensor_scalar(out=nsp16[:], in0=nsp8[:], scalar1=-4.0, scalar2=None, op0=ALU.mult),
              r=["nsp8"], w=["nsp16"])
            A("dve", lambda h: h.tensor_scalar(out=hbrg[:], in0=cf[:, O_BRG:O_BRG + 32], scalar1=0.5, scalar2=None, op0=ALU.mult),
              r=["cf"], w=["hbrg"])
            A("dve", lambda h: h.tensor_scalar(out=hbin[:], in0=cf[:, O_BIN:O_BIN + NWIN], scalar1=0.5, scalar2=None, op0=ALU.mult),
              r=["cf"], w=["hbin"])
            A("dve", lambda h: h.tensor_scalar(out=nsp8[:], in0=nsp8[:], scalar1=-8.0, scalar2=None, op0=ALU.mult),
              r=["nsp8", "nsp16"], w=["nsp8"])
            A("act", lambda h: h.activation(out=esink[:], in_=cf[:, O_SINK:O_SINK + 16], func=AF.Exp),
              r=["cf"], w=["esink"])

            def rmsnorm_to_xn(gidx, dst, dst_tok):
                b, bt = nb()
                for k in range(KC):
                    sq = sqb[k % 2]
                    A("act", lambda h, k=k, sq=sq: h.activation(out=sq[:], in_=hT[:, k, :], func=AF.Square),
                      r=[("h", k)], w=[("sq", k % 2)])
                    A("pe", lambda h, k=k, sq=sq: h.matmul(b[:, :], lhsT=ones_bf, rhs=sq[:], start=(k == 0), stop=(k == KC - 1)),
                      r=[("sq", k % 2), "cbf"], w=[bt])
                A("act", lambda h: h.activation(out=rs[:], in_=b[:, :], func=AF.Sqrt, scale=1.0 / D, bias=EPS),
                  r=[bt], w=["rs"])
                A("dve", lambda h: h.reciprocal(out=rstd[:], in_=rs[:]), r=["rs"], w=["rstd"])
                for k in range(KC):
                    A("dve", lambda h, k=k: h.scalar_tensor_tensor(
                        out=dst[:, k, :], in0=hT[:, k, :], scalar=cf[:, O_GAIN + gidx * 16 + k:O_GAIN + gidx * 16 + k + 1],
                        in1=rstd[:], op0=ALU.mult, op1=ALU.mult),
                      r=[("h", k), "rstd", "cf"], w=[(dst_tok, k)])

            def win_chunk(cidx):
                wt, wtok = wstream(win[cidx])
                b, bt = nb()
                for k in range(KC):
                    A("pe", lambda h, k=k: h.matmul(b[:, :], lhsT=wt[:, k, :], rhs=xn[:, k, :], start=(k == 0), stop=(k == KC - 1)),
                      r=[wtok, ("xn", k)], w=[bt])
                return b, bt

            def bias_col(cidx):
                return cf[:, O_BIN + cidx:O_BIN + cidx + 1]

            def kv_chunks():
                for kvh in range(4):
                    b, bt = win_chunk(C_K + kvh)
                    A("act", lambda h, b=b, kvh=kvh: h.activation(out=kT[:, kvh, 128:128 + NT], in_=b[:, :], func=AF.Identity,
                                                                  bias=bias_col(C_K + kvh)),
                      r=[bt, "cf"], w=["kT"])
                for half in range(2):
                    b, bt = nb()
                    for tb2 in range(2):
                        tb = half * 2 + tb2
                        for k in range(KC):
                            A("pe", lambda h, k=k, tb=tb, tb2=tb2, b=b: h.matmul(
                                b[:, tb2 * 256:(tb2 + 1) * 256], lhsT=xn[:, k, tb * 128:(tb + 1) * 128], rhs=wv[:, k, :],
                                start=(k == 0), stop=(k == KC - 1)),
                              r=[("xn", k), "wv"], w=[bt])
                    for tb2 in range(2):
                        tb = half * 2 + tb2
                        src = b[:, tb2 * 256:(tb2 + 1) * 256].rearrange("p (a c) -> p a c", c=64)
                        bvv = cf[:, O_BV:O_BV + 256].rearrange("p (a c) -> p a c", c=64)
                        A("dve", lambda h, tb=tb, src=src, bvv=bvv: h.tensor_tensor(out=vlo[:, 1 + tb, :, 0:64], in0=src, in1=bvv, op=ALU.add),
                          r=[bt, "cf"], w=["vlo"])
                        A("dve", lambda h, tb=tb, src=src, bvv=bvv: h.tensor_tensor(out=vhi[:, 1 + tb, :, 64:128], in0=src, in1=bvv, op=ALU.add),
                          r=[bt, "cf"], w=["vhi"])

            def kv_halo():
                A("act", lambda h: h.activation(out=kT[:, :, 0:128], in_=kT[:, :, NT:NT + 128], func=AF.Copy), r=["kT"], w=["kT"])
                A("dve", lambda h: h.tensor_copy(out=vlo[:, 0, :, :], in_=vlo[:, 4, :, :]), r=["vlo"], w=["vlo"])
                A("dve", lambda h: h.tensor_copy(out=vhi[:, 0, :, :], in_=vhi[:, 4, :, :]), r=["vhi"], w=["vhi"])

            def lru_block(n, prefix, first_prefix, first_own):
                alt = prefix and (n % 2 == 1)
                if alt:
                    ucL = [gt[0][:], gt[1][:]]
                    ucbL = [pm[0][:], pm[1][:]]
                    uct = [("gt", 0), ("gt", 1)]
                    ucbt = [("pm", 0), ("pm", 1)]
                else:
                    ucL = [uc[:, 0, :], uc[:, 1, :]]
                    ucbL = [ucb[:, 0, :], ucb[:, 1, :]]
                    uct = [("uc", 0), ("uc", 1)]
                    ucbt = [("ucb", 0), ("ucb", 1)]
                wg = wrgt[n % 2]
                A("pool", lambda h: h.dma_start(out=wg[:], in_=wrg_d[n]), w=[("wrg", n % 2)], dma=True)
                for j in range(2):
                    ci = C_LRU + 4 * n + j
                    b, bt = win_chunk(ci)
                    A("act", lambda h, b=b, j=j, ci=ci: h.activation(out=u[:, j, 3:3 + NT], in_=b[:, :], func=AF.Identity, bias=bias_col(ci)),
                      r=[bt, "cf"], w=[("u", j)])
                A("dve", lambda h: h.tensor_copy(out=u[:, :, 0:3], in_=uh[:, 2 * n:2 * n + 2, :]), r=["uh"], w=[("u", 0), ("u", 1)])
                A("dve", lambda h: h.tensor_copy(out=uh[:, 2 * n:2 * n + 2, :], in_=u[:, :, NT:NT + 3]), r=[("u", 0), ("u", 1)], w=["uh"])
                for j in range(2):
                    c = 2 * n + j
                    A("act", lambda h, j=j, c=c: h.activation(out=ucL[j], in_=u[:, j, 0:NT], func=AF.Identity,
                                                             scale=cf[:, O_CONVW + c:O_CONVW + c + 1], bias=cf[:, O_CONVB + c:O_CONVB + c + 1]),
                      r=[("u", j), "cf"], w=[uct[j]])
                for tap in range(1, 4):
                    for j in range(2):
                        c = 2 * n + j
                        A("dve", lambda h, j=j, c=c, tap=tap: h.scalar_tensor_tensor(
                            out=ucL[j], in0=u[:, j, tap:tap + NT], scalar=cf[:, O_CONVW + tap * 16 + c:O_CONVW + tap * 16 + c + 1],
                            in1=ucL[j], op0=ALU.mult, op1=ALU.add),
                          r=[("u", j), uct[j], "cf"], w=[uct[j]])
                for j in range(2):
                    A("act", lambda h, j=j: h.activation(out=ucbL[j], in_=ucL[j], func=AF.Copy), r=[uct[j]], w=[ucbt[j]])
                yield
                gb_ = {}
                for j in range(2):
                    for ax in range(2):
                        bb, btt = nb()
                        gb_[(j, ax)] = (bb, btt)
                        for i in range(2):
                            A("pe", lambda h, ax=ax, bb=bb, i=i, j=j: h.matmul(
                                bb[:, :], lhsT=wg[:, ax, i, j * 128:(j + 1) * 128], rhs=ucbL[i], start=(i == 0), stop=(i == 1)),
                              r=[("wrg", n % 2), ucbt[i]], w=[btt])
                for j in range(2):
                    c = 2 * n + j
                    br, btr = gb_[(j, 0)]
                    bx, btx = gb_[(j, 1)]
                    A("act", lambda h, br=br, c=c, j=j: h.activation(out=rt[j][:], in_=br[:, :], func=AF.Tanh, scale=0.5, bias=hbrg[:, c:c + 1]),
                      r=[btr, "hbrg"], w=[("rt", j)])
                    A("act", lambda h, bx=bx, c=c, j=j: h.activation(out=igt[j][:], in_=bx[:, :], func=AF.Tanh, scale=0.5, bias=hbrg[:, 16 + c:16 + c + 1]),
                      r=[btx, "hbrg"], w=[("igt", j)])
                for j in range(2):
                    c = 2 * n + j
                    A("act", lambda h, c=c, j=j: h.activation(out=at[j][:], in_=rt[j][:], func=AF.Exp, scale=nsp16[:, c:c + 1], bias=nsp16[:, c:c + 1]),
                      r=[("rt", j), "nsp16"], w=[("at", j)])
                    A("act", lambda h, c=c, j=j: h.activation(out=a2t[j][:], in_=rt[j][:], func=AF.Exp, scale=nsp8[:, c:c + 1], bias=nsp8[:, c:c + 1]),
                      r=[("rt", j), "nsp8"], w=[("a2t", j)])
                for j in range(2):
                    A("act", lambda h, j=j: h.activation(out=a2t[j][:], in_=a2t[j][:], func=AF.Sqrt, scale=-1.0, bias=1.0), r=[("a2t", j)], w=[("a2t", j)])
                for j in range(2):
                    if first_prefix:
                        A("dve", lambda h, j=j: h.memset(a2t[j][:, 0:1], 1.0), r=[("a2t", j)], w=[("a2t", j)])
                    if first_own:
                        A("dve", lambda h, j=j: h.tensor_scalar(out=a2t[j][:, 0:1], in0=a2t[j][:, 0:1], scalar1=flags[:, 1:2], scalar2=flags[:, 2:3],
                                                                op0=ALU.mult, op1=ALU.add), r=[("a2t", j), "flags"], w=[("a2t", j)])
                for j in range(2):
                    A("dve", lambda h, j=j: h.scalar_tensor_tensor(out=igt[j][:], in0=igt[j][:], scalar=1.0, in1=ucL[j], op0=ALU.add, op1=ALU.mult),
                      r=[("igt", j), uct[j]], w=[("igt", j)])
                for j in range(2):
                    A("dve", lambda h, j=j: h.scalar_tensor_tensor(out=igt[j][:], in0=igt[j][:], scalar=0.5, in1=a2t[j][:], op0=ALU.mult, op1=ALU.mult),
                      r=[("igt", j), ("a2t", j)], w=[("igt", j)])
                for j in range(2):
                    c = 2 * n + j
                    A("dve", lambda h, c=c, j=j: h.tensor_tensor_scan(out=hseq[j][:], data0=at[j][:], data1=igt[j][:], initial=hstate[:, c:c + 1],
                                                                      op0=ALU.mult, op1=ALU.add), r=[("at", j), ("igt", j), "hstate"], w=[("hseq", j)])
                for j in range(2):
                    c = 2 * n + j
                    A("dve", lambda h, c=c, j=j: h.tensor_copy(out=hstate[:, c:c + 1], in_=hseq[j][:, NT - 1:NT]), r=[("hseq", j)], w=["hstate"])
                yield
                if not prefix:
                    for j in range(2):
                        ci = C_LRU + 4 * n + 2 + j
                        b, bt = win_chunk(ci)
                        A("act", lambda h, b=b, ci=ci, j=j: h.activation(out=gt[j][:], in_=b[:, :], func=AF.Identity, bias=bias_col(ci)),
                          r=[bt, "cf"], w=[("gt", j)])
                        A("act", lambda h, j=j: h.activation(out=t1[j][:], in_=gt[j][:], func=AF.Square), r=[("gt", j)], w=[("t1", j)])
                    for j in range(2):
                        A("dve", lambda h, j=j: h.tensor_scalar(out=t1[j][:], in0=t1[j][:], scalar1=0.044715, scalar2=1.0, op0=ALU.mult, op1=ALU.add),
                          r=[("t1", j)], w=[("t1", j)])
                    for j in range(2):
                        A("dve", lambda h, j=j: h.tensor_tensor(out=t1[j][:], in0=t1[j][:], in1=gt[j][:], op=ALU.mult), r=[("t1", j), ("gt", j)], w=[("t1", j)])
                    for j in range(2):
                        A("act", lambda h, j=j: h.activation(out=t1[j][:], in_=t1[j][:], func=AF.Tanh, scale=0.7978845608028654), r=[("t1", j)], w=[("t1", j)])
                    for j in range(2):
                        A("dve", lambda h, j=j: h.scalar_tensor_tensor(out=t1[j][:], in0=t1[j][:], scalar=1.0, in1=gt[j][:], op0=ALU.add, op1=ALU.mult),
                          r=[("t1", j), ("gt", j)], w=[("t1", j)])
                    for j in range(2):
                        c = 2 * n + j
                        A("dve", lambda h, c=c, j=j: h.scalar_tensor_tensor(out=ylr[:, c, :], in0=t1[j][:], scalar=0.5, in1=hseq[j][:], op0=ALU.mult, op1=ALU.mult),
                          r=[("t1", j), ("hseq", j)], w=[("ylr", c)])
                    yield

            def attention(first_own):
                iters = [(qb, c) for qb in range(4) for c in range(KC)]
                sbanks = {}

                def s_stage(i):
                    qb, c = iters[i]
                    kvh = c // 4
                    p = pm[i % 3]
                    use0 = first_own and qb == 0
                    for hh in range(2):
                        lo, hi = hh * 64, hh * 64 + 64
                        b, bt = nb()
                        if use0:
                            A("pe", lambda h, b=b: h.matmul(b[:, 0:256], lhsT=ident_bf, rhs=mb40[:, 0:256], start=True, stop=False),
                              r=["cbf", "mb40"], w=[bt])
                        else:
                            A("pe", lambda h, b=b: h.matmul(b[:, 0:256], lhsT=ident_bf, rhs=mb4[:, 0:256], start=True, stop=False),
                              r=["cbf"], w=[bt])
                        for pc in range(2):
                            col = pc * 128
                            kb = qb + pc
                            A("pe", lambda h, b=b, lo=lo, hi=hi, col=col, kb=kb, pc=pc: h.matmul(
                                b[:, col:col + 128], lhsT=kT[lo:hi, kvh, kb * 128:(kb + 1) * 128], rhs=qT[lo:hi, c, qb * 128:(qb + 1) * 128],
                                start=False, stop=(pc == 1)),
                              r=["kT", ("q", c)], w=[bt])
                        A("act", lambda h, b=b, hh=hh: h.activation(out=p[:, hh * 256:(hh + 1) * 256], in_=b[:, 0:256], func=AF.Exp, scale=0.125),
                          r=[bt], w=[("pm", i % 3)])

                def pv_stage(i):
                    qb, c = iters[i]
                    kvh = c // 4
                    p = pm[i % 3]
                    b, bt = nb()
                    seq = [(vlo, qb, 0), (vlo, qb + 1, 1), (vhi, qb, 2), (vhi, qb + 1, 3)]
                    for n_, (vt, blk, pc) in enumerate(seq):
                        A("pe", lambda h, vt=vt, blk=blk, pc=pc, n_=n_: h.matmul(
                            b[:, 0:128], lhsT=vt[:, blk, kvh, :], rhs=p[:, pc * 128:(pc + 1) * 128], start=(n_ == 0), stop=(n_ == 3)),
                          r=["vlo", "vhi", ("pm", i % 3)], w=[bt])
                    seq2 = [(oneslo, 0), (oneslo, 1), (oneshi, 2), (oneshi, 3)]
                    for n_, (ot, pc) in enumerate(seq2):
                        A("pe", lambda h, ot=ot, pc=pc, n_=n_: h.matmul(
                            b[:, 128:256], lhsT=ot, rhs=p[:, pc * 128:(pc + 1) * 128], start=(n_ == 0), stop=(n_ == 3)),
                          r=["cbf", ("pm", i % 3)], w=[bt])
                    de = dent[i % 2]
                    re = rect[i % 2]
                    A("dve", lambda h: h.tensor_scalar(out=de[:], in0=b[:, 128:256], scalar1=esink[:, c:c + 1], scalar2=None, op0=ALU.add),
                      r=[bt, "esink"], w=[("dent", i % 2)])
                    A("dve", lambda h: h.reciprocal(out=re[:], in_=de[:]), r=[("dent", i % 2)], w=[("rect", i % 2)])
                    A("dve", lambda h: h.tensor_tensor(out=yat[:, c, qb * 128:(qb + 1) * 128], in0=b[:, 0:128], in1=re[:], op=ALU.mult),
                      r=[bt, ("rect", i % 2)], w=[("yat", c)])

                n_it = len(iters)
                s_stage(0)
                s_stage(1)
                for i in range(n_it):
                    pv_stage(i)
                    if i + 2 < n_it:
                        s_stage(i + 2)
                    if i % 2 == 1:
                        yield

            def merge_and_out(ti):
                mg = qT
                for j in range(KC):
                    jj = j % 2
                    ga, gl, m1, m2 = rt[jj], igt[jj], at[jj], a2t[jj]
                    wa, wat = wstream(wproj[0, j])
                    ba, bat = nb()
                    for k in range(KC):
                        A("pe", lambda h, k=k, wa=wa, ba=ba: h.matmul(ba[:, :], lhsT=wa[:, k, :], rhs=yat[:, k, :], start=(k == 0), stop=(k == KC - 1)),
                          r=[wat, ("yat", k)], w=[bat])
                    wb_, wbt = wstream(wproj[1, j])
                    bb, bbt = nb()
                    for k in range(KC):
                        A("pe", lambda h, k=k, wb_=wb_, bb=bb: h.matmul(bb[:, :], lhsT=wb_[:, k, :], rhs=ylr[:, k, :], start=(k == 0), stop=(k == KC - 1)),
                          r=[wbt, ("ylr", k)], w=[bbt])
                    cga = C_GATE + 2 * j
                    bga, bgat = win_chunk(cga)
                    bgl, bglt = win_chunk(cga + 1)
                    A("act", lambda h, bga=bga, cga=cga, ga=ga: h.activation(out=ga[:], in_=bga[:, :], func=AF.Tanh, scale=0.5, bias=hbin[:, cga:cga + 1]),
                      r=[bgat, "hbin"], w=[("rt", jj)])
                    A("act", lambda h, bgl=bgl, cga=cga, gl=gl: h.activation(out=gl[:], in_=bgl[:, :], func=AF.Tanh, scale=0.5, bias=hbin[:, cga + 1:cga + 2]),
                      r=[bglt, "hbin"], w=[("igt", jj)])
                    A("dve", lambda h, ba=ba, ga=ga, m1=m1: h.scalar_tensor_tensor(out=m1[:], in0=ga[:], scalar=1.0, in1=ba[:, :], op0=ALU.add, op1=ALU.mult),
                      r=[bat, ("rt", jj)], w=[("at", jj)])
                    A("dve", lambda h, bb=bb, gl=gl, m2=m2: h.scalar_tensor_tensor(out=m2[:], in0=gl[:], scalar=1.0, in1=bb[:, :], op0=ALU.add, op1=ALU.mult),
                      r=[bbt, ("igt", jj)], w=[("a2t", jj)])
                    A("dve", lambda h, j=j, m1=m1, m2=m2: h.tensor_tensor(out=mg[:, j, :], in0=m1[:], in1=m2[:], op=ALU.add),
                      r=[("at", jj), ("a2t", jj)], w=[("q", j)])
                for j in range(KC):
                    wo, wot = wstream(wproj[2, j])
                    bo, bot = nb()
                    for k in range(KC):
                        A("pe", lambda h, k=k, wo=wo, bo=bo: h.matmul(bo[:, :], lhsT=wo[:, k, :], rhs=mg[:, k, :], start=(k == 0), stop=(k == KC - 1)),
                          r=[wot, ("q", k)], w=[bot])
                    A("dve", lambda h, j=j, bo=bo: h.scalar_tensor_tensor(out=hT[:, j, :], in0=bo[:, :], scalar=0.5, in1=hT[:, j, :], op0=ALU.mult, op1=ALU.add),
                      r=[bot, ("h", j)], w=[("h", j)])
                for half in range(2):
                    A("sp", lambda h, half=half: h.dma_start(out=hmid[:, half * 8:(half + 1) * 8, ti * NT:(ti + 1) * NT],
                                                            in_=hT[:, half * 8:(half + 1) * 8, :]),
                      r=[("h", k) for k in range(half * 8, half * 8 + 8)], w=[("hmid", ti, half)], dma=True)

            def route_and_dispatch(ti):
                rmsnorm_to_xn(1, xn, "xn")
                for tb in range(4):
                    blk = ti * 4 + tb
                    b, bt = nb()
                    for k in range(KC):
                        A("pe", lambda h, k=k, tb=tb, b=b: h.matmul(b[:, 0:NE], lhsT=xn[:, k, tb * 128:(tb + 1) * 128], rhs=wrt[:, k, :],
                                                                    start=(k == 0), stop=(k == KC - 1)),
                          r=[("xn", k), "wrt"], w=[bt])
                    A("dve", lambda h, b=b: h.tensor_tensor(out=lg[:], in0=b[:, 0:NE], in1=cf[:, O_BROUT:O_BROUT + NE], op=ALU.add),
                      r=[bt, "cf"], w=["lg"])
                    A("dve", lambda h: h.max(out=top8[:], in_=lg[:]), r=["lg"], w=["top8"])
                    A("dve", lambda h: h.tensor_scalar(out=ntop[:], in0=top8[:, 0:1], scalar1=-1.0, scalar2=None, op0=ALU.mult),
                      r=["top8"], w=["ntop"])
                    A("dve", lambda h: h.tensor_scalar(out=mk[:], in0=lg[:], scalar1=top8[:, 3:4], scalar2=None, op0=ALU.is_ge),
                      r=["lg", "top8"], w=["mk"])
                    A("dve", lambda h: h.tensor_copy(out=mkb[:], in_=mk[:]), r=["mk"], w=["mkb"])
                    br_, brt = nb()
                    A("pe", lambda h, br_=br_: h.matmul(br_[:, 0:NE], lhsT=lt_bf, rhs=mkb[:], start=True, stop=True), r=["cbf", "mkb"], w=[brt])
                    A("pe", lambda h, br_=br_: h.matmul(br_[:, 64:64 + NE], lhsT=ones_bf, rhs=mkb[:], start=True, stop=True), r=["cbf", "mkb"], w=[brt])
                    A("dve", lambda h, br_=br_: h.tensor_tensor(out=rk[:], in0=br_[:, 0:NE], in1=cntb[:], op=ALU.add), r=[brt, "cntb"], w=["rk"])
                    A("dve", lambda h, br_=br_: h.tensor_tensor(out=cntb[:], in0=br_[:, 64:64 + NE], in1=cntb[:], op=ALU.add), r=[brt, "cntb", "rk"], w=["cntb"])
                    A("dve", lambda h: h.tensor_scalar(out=okr[:], in0=rk[:], scalar1=float(CAP), scalar2=None, op0=ALU.is_lt), r=["rk"], w=["okr"])
                    A("dve", lambda h: h.tensor_tensor(out=okr[:], in0=okr[:], in1=mk[:], op=ALU.mult), r=["okr", "mk"], w=["okr"])
                    A("dve", lambda h: h.tensor_tensor(out=rk[:], in0=rk[:], in1=cf[:, O_ECAP:O_ECAP + NE], op=ALU.add), r=["rk", "cf"], w=["rk"])
                    A("dve", lambda h: h.tensor_tensor(out=rk[:], in0=rk[:], in1=okr[:], op=ALU.mult), r=["rk", "okr"], w=["rk"])
                    A("dve", lambda h: h.tensor_scalar(out=rk[:], in0=rk[:], scalar1=BIG, scalar2=None, op0=ALU.add), r=["rk"], w=["rk"])
                    for kk in range(4):
                        A("dve", lambda h, kk=kk, blk=blk: h.scalar_tensor_tensor(
                            out=junk[:], in0=lg[:], scalar=top8[:, kk:kk + 1], in1=rk[:], op0=ALU.is_equal, op1=ALU.mult,
                            accum_out=dest_f[:, blk * 4 + kk:blk * 4 + kk + 1]),
                          r=["lg", "top8", "rk", "junk"], w=["junk", ("dest_f", blk)])
                    A("dve", lambda h, blk=blk: h.tensor_copy(out=dest_i[:, blk * 4:blk * 4 + 4], in_=dest_f[:, blk * 4:blk * 4 + 4]),
                      r=[("dest_f", blk)], w=[("dest_i", blk)])
                    A("act", lambda h: h.activation(out=ex4[:], in_=top8[:, 0:4], func=AF.Exp, bias=ntop[:, 0:1], accum_out=s4[:]),
                      r=["top8", "ntop"], w=["ex4", "s4"])
                    A("dve", lambda h: h.reciprocal(out=s4[:], in_=s4[:]), r=["s4"], w=["s4"])
                    A("dve", lambda h, blk=blk: h.tensor_scalar(out=w4[:, blk * 4:blk * 4 + 4], in0=ex4[:], scalar1=s4[:, 0:1], scalar2=None, op0=ALU.mult),
                      r=["ex4", "s4"], w=[("w4", blk)])
                    A("dve", lambda h, blk=blk: h.tensor_scalar(out=ex4[:], in0=dest_f[:, blk * 4:blk * 4 + 4], scalar1=BIG, scalar2=None, op0=ALU.is_lt),
                      r=[("dest_f", blk), "ex4"], w=["ex4"])
                    A("dve", lambda h, blk=blk: h.tensor_tensor(out=w4[:, blk * 4:blk * 4 + 4], in0=w4[:, blk * 4:blk * 4 + 4], in1=ex4[:], op=ALU.mult),
                      r=["ex4", ("w4", blk)], w=[("w4", blk)])
                    for half in range(2):
                        bT, bTt = nb()
                        bv_ = bT[:].bitcast(BF16)
                        for kq in range(8):
                            k = half * 8 + kq
                            A("pe", lambda h, k=k, kq=kq, tb=tb, bv_=bv_: h.transpose(out=bv_[:, kq * 128:(kq + 1) * 128],
                                                                                      in_=xn[:, k, tb * 128:(tb + 1) * 128], identity=ident_bf),
                              r=[("xn", k), "cbf"], w=[bTt])
                        A("act", lambda h, half=half, bv_=bv_: h.activation(out=xtok[:, half * 1024:(half + 1) * 1024], in_=bv_[:, 0:1024], func=AF.Copy),
                          r=[bTt], w=["xtok"])
                    for kk in range(4):
                        A("pool", lambda h, kk=kk, blk=blk: h.indirect_dma_start(
                            out=xg, out_offset=bass.IndirectOffsetOnAxis(ap=dest_i[:, blk * 4 + kk:blk * 4 + kk + 1], axis=0),
                            in_=xtok[:, :], in_offset=None, bounds_check=P.bc_reg, oob_is_err=False),
                          r=["xtok", ("dest_i", blk)], w=["xg"], dma=True)

            def load_tile(tok0):
                for half in range(2):
                    A("sp", lambda h, half=half: h.dma_start(out=hT[:, half * 8:(half + 1) * 8, :],
                                                            in_=xT[:, half * 8:(half + 1) * 8, tok0:tok0 + NT]),
                      w=[("h", k) for k in range(half * 8, half * 8 + 8)], dma=True)

            for pt in range(NTILE - n_prefix, NTILE):
                load_tile(pt * NT)
                rmsnorm_to_xn(0, xn, "xn")
                if pt == NTILE - 1:
                    kv_chunks()
                    kv_halo()
                for n in range(8):
                    for _ in lru_block(n, True, pt == 0, False):
                        pass
            A("dve", lambda h: h.tensor_scalar(out=hstate[:], in0=hstate[:], scalar1=flags[:, 0:1], scalar2=None, op0=ALU.mult),
              r=["hstate", "flags"], w=["hstate"])
            A("dve", lambda h: h.tensor_scalar(out=uh[:], in0=uh[:], scalar1=flags[:, 0:1], scalar2=None, op0=ALU.mult),
              r=["uh", "flags"], w=["uh"])
            for ti in range(n_own):
                load_tile(TOWN + ti * NT)
                if STAGE < 1:
                    continue
                rmsnorm_to_xn(0, xn, "xn")
                if STAGE < 2:
                    continue
                for c in range(KC):
                    b, bt = win_chunk(C_Q + c)
                    A("act", lambda h, b=b, c=c: h.activation(out=qT[:, c, :], in_=b[:, :], func=AF.Identity, bias=bias_col(C_Q + c)),
                      r=[bt, "cf"], w=[("q", c)])
                if STAGE < 3:
                    continue
                kv_chunks()
                if STAGE < 4:
                    continue
                def lru_all(ti=ti):
                    for n in range(8):
                        yield from lru_block(n, False, False, ti == 0)
                gens = [attention(ti == 0), lru_all()]
                while gens:
                    for g in list(gens):
                        try:
                            next(g)
                        except StopIteration:
                            gens.remove(g)
                if debug and ti == 0:
                    for nm, tl, rr in (("dbg_yat", yat, [("yat", k) for k in range(KC)]), ("dbg_q", qT, [("q", k) for k in range(KC)]),
                                       ("dbg_k", kT, ["kT"]), ("dbg_vlo", vlo, ["vlo"]), ("dbg_vhi", vhi, ["vhi"])):
                        dd = nc.dram_tensor(nm, list(tl.shape), BF16, kind="ExternalOutput").ap()
                        A("sp", lambda h, dd=dd, tl=tl: h.dma_start(out=dd, in_=tl[:]), r=rr, w=[nm], dma=True)
                kv_halo()
                if STAGE < 6:
                    continue
                merge_and_out(ti)
                if STAGE < 7:
                    continue
                route_and_dispatch(ti)
            barrier()
            P.flush()

        with contextlib.ExitStack() as st:
            def sb(name, shape, dt):
                return st.enter_context(nc.sbuf_tensor(name, list(shape), dt))

            xte = [sb("xte%d" % i, [128, NJT, D], BF16) for i in range(2)]
            xgT = [sb("xgT%d" % i, [128, KC, CAP], BF16) for i in range(2)]
            actT = [sb("actT%d" % i, [128, KC, CAP], BF16) for i in range(2)]
            w2b = [sb("w2b%d" % i, [128, KC, 512], BF16) for i in range(2)]
            ytok = [sb("ytok%d" % i, [128, NJT, D], F32) for i in range(2)]
            b2r = [sb("b2r%d" % i, [1, D], BF16) for i in range(2)]
            glt = [sb("glt%d" % i, [128, CAP], F32) for i in range(2)]
            sgt = [sb("sgt%d" % i, [128, CAP], F32) for i in range(2)]
            lnt = [sb("lnt%d" % i, [128, CAP], F32) for i in range(2)]
            w2rr = [0]

            for e in range(n_exp):
                s = e % 2
                xt_ = xte[s]
                for jt in range(NJT):
                    A("sp", lambda h, jt=jt, xt_=xt_, e=e: h.dma_start(out=xt_[0:JR[jt], jt, :], in_=xg[e * CAP + jt * 128:e * CAP + jt * 128 + JR[jt], :]),
                      r=["xg"], w=[("xte", s, jt)], dma=True)
                A("pool", lambda h, e=e, s=s: h.dma_start(out=b2r[s][:], in_=b2_d[e:e + 1, :]), w=[("b2r", s)], dma=True)
                xg_ = xgT[s]
                for jt in range(NJT):
                    for half in range(2):
                        bT, bTt = nb()
                        bv_ = bT[:].bitcast(BF16)
                        for kq in range(8):
                            k = half * 8 + kq
                            A("pe", lambda h, k=k, kq=kq, jt=jt, bv_=bv_, xt_=xt_: h.transpose(
                                out=bv_[:, kq * 128:kq * 128 + JR[jt]], in_=xt_[0:JR[jt], jt, k * 128:(k + 1) * 128],
                                identity=ident_bf[0:JR[jt], 0:JR[jt]]),
                              r=[("xte", s, jt), "cbf"], w=[bTt])
                        A("act", lambda h, half=half, jt=jt, bv_=bv_, xg_=xg_: h.activation(
                            out=xg_[:, half * 8:(half + 1) * 8, jt * 128:jt * 128 + JR[jt]],
                            in_=bv_[:, 0:1024].rearrange("p (a c) -> p a c", c=128)[:, :, 0:JR[jt]], func=AF.Copy),
                          r=[bTt], w=[("xgT", s)])
                at_ = actT[s]
                for i in range(KC):
                    bks = []
                    for fc in (i, KC + i):
                        wt, wtok = wstream(w1_d[e, fc])
                        b, bt = nb()
                        for k in range(KC):
                            A("pe", lambda h, k=k, wt=wt, b=b, xg_=xg_: h.matmul(b[:, 0:CAP], lhsT=wt[:, k, :], rhs=xg_[:, k, :],
                                                                                start=(k == 0), stop=(k == KC - 1)),
                              r=[wtok, ("xgT", s)], w=[bt])
                        bks.append((b, bt))
                    (bg, bgt), (bl, blt) = bks
                    g_, s_, l_ = glt[i % 2], sgt[i % 2], lnt[i % 2]
                    cg = O_B1 + e * 32 + i
                    cl = O_B1 + e * 32 + KC + i
                    A("dve", lambda h, bg=bg, g_=g_, cg=cg: h.tensor_scalar(out=g_[:], in0=bg[:, 0:CAP], scalar1=cf[:, cg:cg + 1], scalar2=7.0,
                                                                            op0=ALU.add, op1=ALU.min),
                      r=[bgt, "cf"], w=[("glt", i % 2)])
                    A("act", lambda h, g_=g_, s_=s_: h.activation(out=s_[:], in_=g_[:], func=AF.Sigmoid, scale=1.702),
                      r=[("glt", i % 2)], w=[("sgt", i % 2)])
                    A("dve", lambda h, bl=bl, l_=l_, cl=cl: h.tensor_scalar(out=l_[:], in0=bl[:, 0:CAP], scalar1=cf[:, cl:cl + 1], scalar2=7.0,
                                                                            op0=ALU.add, op1=ALU.min),
                      r=[blt, "cf"], w=[("lnt", i % 2)])
                    A("dve", lambda h, l_=l_: h.tensor_scalar(out=l_[:], in0=l_[:], scalar1=-7.0, scalar2=1.0, op0=ALU.max, op1=ALU.add),
                      r=[("lnt", i % 2)], w=[("lnt", i % 2)])
                    A("dve", lambda h, g_=g_, s_=s_: h.tensor_tensor(out=g_[:], in0=g_[:], in1=s_[:], op=ALU.mult),
                      r=[("glt", i % 2), ("sgt", i % 2)], w=[("glt", i % 2)])
                    A("dve", lambda h, g_=g_, l_=l_, i=i, at_=at_: h.tensor_tensor(out=at_[:, i, :], in0=g_[:], in1=l_[:], op=ALU.mult),
                      r=[("glt", i % 2), ("lnt", i % 2)], w=[("actT", s, i)])
                yt_ = ytok[s]
                for dg in range(4):
                    ws = w2rr[0] % 2
                    w2rr[0] += 1
                    w2t = w2b[ws]
                    A("pool", lambda h, w2t=w2t, e=e, dg=dg: h.dma_start(out=w2t[:], in_=w2_d[e, dg], max_dma_last_dim=4096),
                      w=[("w2b", ws)], dma=True)
                    for jt in range(NJT):
                        b, bt = nb()
                        for k in range(KC):
                            A("pe", lambda h, k=k, jt=jt, b=b, w2t=w2t, at_=at_: h.matmul(
                                b[0:JR[jt], :], lhsT=at_[:, k, jt * 128:jt * 128 + JR[jt]], rhs=w2t[:, k, :], start=(k == 0), stop=False),
                              r=[("actT", s, k), ("w2b", ws)], w=[bt])
                        A("pe", lambda h, b=b, dg=dg, s=s, jt=jt: h.matmul(b[0:JR[jt], :], lhsT=ones_bf[0:1, 0:JR[jt]],
                                                                           rhs=b2r[s][0:1, dg * 512:(dg + 1) * 512], start=False, stop=True),
                          r=["cbf", ("b2r", s)], w=[bt])
                        A("act", lambda h, b=b, jt=jt, dg=dg, yt_=yt_: h.activation(out=yt_[0:JR[jt], jt, dg * 512:(dg + 1) * 512], in_=b[0:JR[jt], :],
                                                                                    func=AF.Copy),
                          r=[bt], w=[("ytok", s, jt)])
                for jt in range(NJT):
                    A("sp", lambda h, jt=jt, yt_=yt_, e=e: h.dma_start(out=yg[e * CAP + jt * 128:e * CAP + jt * 128 + JR[jt], :], in_=yt_[0:JR[jt], jt, :]),
                      r=[("ytok", s, jt)], w=["yg"], dma=True)
            barrier()
            P.flush()

        with contextlib.ExitStack() as st:
            def sb(name, shape, dt):
                return st.enter_context(nc.sbuf_tensor(name, list(shape), dt))

            hT = sb("hT_c", [128, KC, NT], F32)
            xn = sb("xn_c", [128, KC, NT], BF16)
            gb = [sb("gb%d" % i, [128, D], F32) for i in range(4)]
            acc = sb("acc", [128, D], F32)
            wple = sb("wple_t", [128, 2, D], BF16)
            ptb = sb("ptb", [128, 2, NT], BF16)
            sqb = [sb("sqb_c%d" % i, [128, NT], BF16) for i in range(2)]
            rs = sb("rs_c", [128, NT], F32)
            rstd = sb("rstd_c", [128, NT], F32)
            gate = [sb("gate%d" % i, [128, NT], F32) for i in range(2)]
            tmp = [sb("tmp%d" % i, [128, NT], F32) for i in range(2)]

            A("pool", lambda h: h.dma_start(out=wple[:], in_=wple_d, max_dma_last_dim=4096), w=["wple"], dma=True)
            for kk in range(4):
                A("dve", lambda h, kk=kk: h.memset(gb[kk][:], 0.0), w=[("gb", kk)])

            def norm_c(gidx, dst_fn, dst_tok):
                b, bt = nb()
                for k in range(KC):
                    sq = sqb[k % 2]
                    A("act", lambda h, k=k, sq=sq: h.activation(out=sq[:], in_=hT[:, k, :], func=AF.Square),
                      r=[("hc", k)], w=[("sqc", k % 2)])
                    A("pe", lambda h, k=k, sq=sq: h.matmul(b[:, :], lhsT=ones_bf, rhs=sq[:], start=(k == 0), stop=(k == KC - 1)),
                      r=[("sqc", k % 2), "cbf"], w=[bt])
                A("act", lambda h: h.activation(out=rs[:], in_=b[:, :], func=AF.Sqrt, scale=1.0 / D, bias=EPS), r=[bt], w=["rsc"])
                A("dve", lambda h: h.reciprocal(out=rstd[:], in_=rs[:]), r=["rsc"], w=["rstdc"])
                for k in range(KC):
                    A("dve", lambda h, k=k: h.scalar_tensor_tensor(
                        out=dst_fn(k), in0=hT[:, k, :], scalar=cf[:, O_GAIN + gidx * 16 + k:O_GAIN + gidx * 16 + k + 1],
                        in1=rstd[:], op0=ALU.mult, op1=ALU.mult),
                      r=[("hc", k), "rstdc", "cf"], w=[(dst_tok, k)])

            for ti in range(n_own if final else 0):
                for half in range(2):
                    A("sp", lambda h, half=half, ti=ti: h.dma_start(out=hT[:, half * 8:(half + 1) * 8, :],
                                                                    in_=hmid[:, half * 8:(half + 1) * 8, ti * NT:(ti + 1) * NT]),
                      r=[("hmid", ti, half)], w=[("hc", k) for k in range(half * 8, half * 8 + 8)], dma=True)
                A("pool", lambda h, ti=ti: h.dma_start(out=ptb[:], in_=pT[:, :, ti * NT:(ti + 1) * NT]), w=["ptb"], dma=True)
                for tb in range(4):
                    blk = ti * 4 + tb
                    for kk in range(4):
                        A("pool", lambda h, kk=kk, blk=blk: h.indirect_dma_start(
                            out=gb[kk][:, :], out_offset=None, in_=yg,
                            in_offset=bass.IndirectOffsetOnAxis(ap=dest_i[:, blk * 4 + kk:blk * 4 + kk + 1], axis=0),
                            bounds_check=P.bc_reg, oob_is_err=False),
                          r=["yg", ("dest_i", blk)], w=[("gb", kk)], dma=True)
                    A("dve", lambda h, blk=blk: h.tensor_scalar(out=acc[:], in0=gb[0][:], scalar1=w4[:, blk * 4:blk * 4 + 1], scalar2=None, op0=ALU.mult),
                      r=[("gb", 0), ("w4", blk)], w=["acc"])
                    for kk in range(1, 4):
                        A("dve", lambda h, kk=kk, blk=blk: h.scalar_tensor_tensor(
                            out=acc[:], in0=gb[kk][:], scalar=w4[:, blk * 4 + kk:blk * 4 + kk + 1], in1=acc[:], op0=ALU.mult, op1=ALU.add),
                          r=[("gb", kk), ("w4", blk), "acc"], w=["acc"])
                    for q4 in range(4):
                        b, bt = nb()
                        for kq in range(4):
                            k = q4 * 4 + kq
                            A("pe", lambda h, k=k, kq=kq, b=b: h.transpose(out=b[:, kq * 128:(kq + 1) * 128], in_=acc[:, k * 128:(k + 1) * 128],
                                                                           identity=ident_f),
                              r=["acc", "cf"], w=[bt])
                        A("dve", lambda h, q4=q4, tb=tb, b=b: h.tensor_tensor(
                            out=hT[:, q4 * 4:(q4 + 1) * 4, tb * 128:(tb + 1) * 128], in0=b[:, :].rearrange("p (a c) -> p a c", c=128),
                            in1=hT[:, q4 * 4:(q4 + 1) * 4, tb * 128:(tb + 1) * 128], op=ALU.add),
                          r=[bt] + [("hc", k) for k in range(q4 * 4, q4 * 4 + 4)], w=[("hc", k) for k in range(q4 * 4, q4 * 4 + 4)])
                norm_c(2, lambda k: xn[:, k, :], "xnc")
                for j in range(KC):
                    wg_, wgt = wstream(wproj[3, j])
                    bg, bgt = nb()
                    for k in range(KC):
                        A("pe", lambda h, k=k, wg_=wg_, bg=bg: h.matmul(bg[:, :], lhsT=wg_[:, k, :], rhs=xn[:, k, :], start=(k == 0), stop=(k == KC - 1)),
                          r=[wgt, ("xnc", k)], w=[bgt])
                    bp, bpt = nb()
                    for k in range(2):
                        A("pe", lambda h, k=k, j=j, bp=bp: h.matmul(bp[:, :], lhsT=wple[:, k, j * 128:(j + 1) * 128], rhs=ptb[:, k, :],
                                                                    start=(k == 0), stop=(k == 1)),
                          r=["wple", "ptb"], w=[bpt])
                    g_ = gate[j % 2]
                    t_ = tmp[j % 2]
                    A("act", lambda h, bg=bg, g_=g_: h.activation(out=g_[:], in_=bg[:, :], func=AF.Sigmoid), r=[bgt], w=[("gate", j % 2)])
                    A("dve", lambda h, bp=bp, g_=g_, t_=t_: h.tensor_tensor(out=t_[:], in0=bp[:, :], in1=g_[:], op=ALU.mult),
                      r=[bpt, ("gate", j % 2)], w=[("tmp", j % 2)])
                    A("dve", lambda h, j=j, t_=t_: h.tensor_tensor(out=hT[:, j, :], in0=hT[:, j, :], in1=t_[:], op=ALU.add),
                      r=[("tmp", j % 2), ("hc", j)], w=[("hc", j)])
                norm_c(3, lambda k: hT[:, k, :], "hc")
                for half in range(2):
                    A("sp", lambda h, half=half, ti=ti: h.dma_start(out=outT[:, half * 8:(half + 1) * 8, ti * NT:(ti + 1) * NT],
                                                                    in_=hT[:, half * 8:(half + 1) * 8, :]),
                      r=[("hc", k) for k in range(half * 8, half * 8 + 8)], w=[("out", ti, half)], dma=True)
            P.flush(final=True)
    return nc


def _chunk_w(w):
    K, M = w.shape
    return np.ascontiguousarray(w.reshape(KC, 128, M // 128, 128).transpose(2, 1, 0, 3))


def _pcol(v):
    return np.ascontiguousarray(v.reshape(-1, 128).T)


def _host_layout(inp):
    f = np.float32
    w_in = inp["w_in"][0]
    b_in = inp["b_in"][0]
    QW, KVW = 2048, 256
    oq, ok, ov = 0, QW, QW + KVW
    ou = QW + 2 * KVW
    og = ou + 2048
    oga = og + 2048
    ogl = oga + 2048
    cols = []
    for c in range(16):
        cols.append(np.arange(oq + c * 128, oq + (c + 1) * 128))
    for kvh in range(4):
        a = np.arange(ok + kvh * 64, ok + (kvh + 1) * 64)
        cols.append(np.concatenate([a, a]))
    for n in range(8):
        cols.append(np.arange(ou + (2 * n) * 128, ou + (2 * n + 1) * 128))
        cols.append(np.arange(ou + (2 * n + 1) * 128, ou + (2 * n + 2) * 128))
        cols.append(np.arange(og + (2 * n) * 128, og + (2 * n + 1) * 128))
        cols.append(np.arange(og + (2 * n + 1) * 128, og + (2 * n + 2) * 128))
    for j in range(16):
        cols.append(np.arange(oga + j * 128, oga + (j + 1) * 128))
        cols.append(np.arange(ogl + j * 128, ogl + (j + 1) * 128))
    cols = np.concatenate(cols)
    assert cols.size == NWIN * 128
    win = _chunk_w(w_in[:, cols])
    bin_ = _pcol(b_in[cols])
    wv = np.ascontiguousarray(w_in[:, ov:ov + 256].reshape(KC, 128, 256).transpose(1, 0, 2))
    bv = np.broadcast_to(b_in[ov:ov + 256][None, :], (128, 256))
    wproj = np.stack([_chunk_w(inp["w_attn_proj"][0]), _chunk_w(inp["w_lru_proj"][0]),
                      _chunk_w(inp["w_out"][0]), _chunk_w(inp["w_ple_gate"][0])])
    wa = inp["w_rg_a"][0].reshape(8, 2, 128, 256).transpose(0, 2, 1, 3)
    wx = inp["w_rg_x"][0].reshape(8, 2, 128, 256).transpose(0, 2, 1, 3)
    wrg = np.ascontiguousarray(np.stack([wa, wx], axis=2))
    brg = np.concatenate([_pcol(inp["b_rg_a"][0].reshape(-1)), _pcol(inp["b_rg_x"][0].reshape(-1))], axis=1)
    convw = np.concatenate([_pcol(inp["conv_w"][0][t]) for t in range(4)], axis=1)
    convb = _pcol(inp["conv_b"][0])
    lam = _pcol(inp["lru_lambda"][0])
    gains = np.concatenate([_pcol(inp["norm_mix_g"][0]), _pcol(inp["norm_ffn_g"][0]),
                            _pcol(inp["norm_ple_g"][0]), _pcol(inp["norm_final_g"])], axis=1)
    sink = _pcol(np.repeat(inp["attn_sinks"][0], 64))
    wrouter = np.ascontiguousarray(inp["w_router"][0].reshape(KC, 128, NE).transpose(1, 0, 2))
    brout = np.broadcast_to(inp["b_router"][0][None, :], (128, NE))
    wple = np.ascontiguousarray(inp["w_ple"][0].reshape(2, 128, D).transpose(1, 0, 2))
    w1 = inp["w_mlp1"][0]
    w1l = np.ascontiguousarray(w1.reshape(NE, KC, 128, 32, 128).transpose(0, 3, 2, 1, 4))
    b1 = inp["b_mlp1"][0].reshape(NE, 32, 128).transpose(2, 0, 1).reshape(128, NE * 32)
    w2 = inp["w_mlp2"][0]
    w2l = np.ascontiguousarray(w2.reshape(NE, KC, 128, 4, 512).transpose(0, 3, 2, 1, 4))
    b2 = np.ascontiguousarray(inp["b_mlp2"][0])
    ecap = np.broadcast_to((np.arange(NE, dtype=f) * CAP - BIG)[None, :], (128, NE))
    cf = np.zeros((128, NCF), f)
    cf[:, O_IDF:O_IDF + 128] = np.eye(128, dtype=f)
    cf[:, O_ECAP:O_ECAP + NE] = ecap
    cf[:, O_BROUT:O_BROUT + NE] = brout
    cf[:, O_BV:O_BV + 256] = bv
    cf[:, O_BIN:O_BIN + NWIN] = bin_
    cf[:, O_BRG:O_BRG + 32] = brg
    cf[:, O_CONVW:O_CONVW + 64] = convw
    cf[:, O_CONVB:O_CONVB + 16] = convb
    cf[:, O_LAM:O_LAM + 16] = lam
    cf[:, O_GAIN:O_GAIN + 64] = gains
    cf[:, O_SINK:O_SINK + 16] = sink
    cf[:, O_B1:O_B1 + 1024] = b1
    NEG = -30000.0
    jj = np.arange(128)[:, None]
    ii = np.arange(128)[None, :]
    mbprev = np.where(jj > ii, 0.0, NEG).astype(f)
    mbcur = np.where(jj <= ii, 0.0, NEG).astype(f)
    cbf = np.zeros((128, NCB), f)
    cbf[:, B_ID:B_ID + 128] = np.eye(128, dtype=f)
    cbf[:, B_LT:B_LT + 128] = (jj < ii).astype(f)
    cbf[:, B_ONES:B_ONES + 128] = 1.0
    cbf[:, B_MB4:B_MB4 + 512] = np.concatenate([mbprev, mbcur, mbprev, mbcur], axis=1)
    cbf[:, B_OLO:B_OLO + 64] = 1.0
    cbf[:, B_OHI + 64:B_OHI + 128] = 1.0
    shared = dict(cbf=cbf, cf=cf, win=win, wv=wv, wproj=wproj, wrg=wrg, wrouter=wrouter, wple=wple,
                  w1=w1l, w2=w2l, b2=b2)
    x = inp["x"]
    p = inp["p"][0]
    in_maps = []
    for core in range(NCORES):
        b, half = core // 2, core % 2
        xt = x[b].T.reshape(KC, 128, 2 * TOWN).transpose(1, 0, 2)
        if half == 0:
            xT = np.concatenate([np.zeros((128, KC, TOWN), f), xt[:, :, :TOWN]], axis=2)
        else:
            xT = xt
        pt = p[b, half * TOWN:(half + 1) * TOWN].T.reshape(2, 128, TOWN).transpose(1, 0, 2)
        ccv = np.zeros((128, 516), f)
        mb0 = np.full((128, 128), NEG, f) if half == 0 else mbprev
        ccv[:, 0:512] = np.concatenate([mb0, mbcur, mb0, mbcur], axis=1)
        ccv[:, 512] = float(half)
        ccv[:, 513] = float(half)
        ccv[:, 514] = float(1 - half)
        m = dict(shared)
        m["xT"] = np.ascontiguousarray(xT, dtype=f)
        m["pT"] = np.ascontiguousarray(pt, dtype=f)
        m["cc"] = ccv
        in_maps.append(m)
    return in_maps


_NC_CACHE = {}


def kernel(**inputs):
    inp = {k: np.asarray(v) for k, v in inputs.items()}
    in_maps = _host_layout(inp)
    if "nc" not in _NC_CACHE:
        _NC_CACHE["nc"] = build_nc()
    nc = _NC_CACHE["nc"]
    res = run_bass_kernel_spmd(nc, in_maps, core_ids=list(range(NCORES)))
    out = np.empty((4, 4096, D), np.float32)
    for core in range(NCORES):
        b, half = core // 2, core % 2
        o = res.results[core]["outT"]
        out[b, half * TOWN:(half + 1) * TOWN, :] = o.transpose(2, 1, 0).reshape(TOWN, D)
    return out
```

```python
import contextlib
import numpy as np
import concourse.bass as bass
import concourse.mybir as mybir
from concourse.bass_utils import run_bass_kernel_spmd

F32 = mybir.dt.float32
BF16 = mybir.dt.bfloat16
I32 = mybir.dt.int32
ALU = mybir.AluOpType
AF = mybir.ActivationFunctionType

D = 2048
KC = 16
NT = 512
TOWN = 2048
NTILE = TOWN // NT
NE = 32
CAP = 352
NJT = 3
JR = (128, 128, 96)
BIG = 65536.0
EPS = 1e-6
NCORES = 8
STAGE = 99

C_Q = 0
C_K = 16
C_LRU = 20
C_GATE = 52
NWIN = 84

O_IDF = 0
O_ECAP = 128
O_BROUT = 160
O_BV = 192
O_BIN = 448
O_BRG = O_BIN + NWIN
O_CONVW = O_BRG + 32
O_CONVB = O_CONVW + 64
O_LAM = O_CONVB + 16
O_GAIN = O_LAM + 16
O_SINK = O_GAIN + 64
O_B1 = O_SINK + 16
NCF = O_B1 + 1024
B_ID = 0
B_LT = 128
B_ONES = 256
B_MB4 = 384
B_OLO = 896
B_OHI = 1024
NCB = 1152

ENGS = ("pe", "act", "dve", "pool", "sp")


class Op:
    __slots__ = ("eng", "fn", "deps", "is_dma", "sem", "cnt", "signal", "sig_idx")


class Prog:
    def __init__(self, nc, n_dma_sems=8):
        self.nc = nc
        self.ops = {e: [] for e in ENGS}
        self.last_w = {}
        self.readers = {}
        self.n_dma_sems = n_dma_sems
        self.dma_rr = {e: 0 for e in ENGS}
        self.dma_last = {}
        self.dma_cnt = {}

    def add(self, eng, fn, r=(), w=(), dma=False, extra=()):
        op = Op()
        op.eng = eng; op.fn = fn; op.is_dma = dma; op.signal = False; op.sig_idx = 0
        op.sem = None; op.cnt = 0
        deps = list(extra)
        for t in r:
            x = self.last_w.get(t)
            if x is not None:
                deps.append(x)
        for t in w:
            x = self.last_w.get(t)
            if x is not None:
                deps.append(x)
            deps.extend(self.readers.get(t, ()))
        if dma:
            slot = self.dma_rr[eng] % self.n_dma_sems
            self.dma_rr[eng] += 1
            key = (eng, slot)
            prev = self.dma_last.get(key)
            if prev is not None:
                deps.append(prev)
            self.dma_last[key] = op
            self.dma_cnt[key] = self.dma_cnt.get(key, 0) + 16
            op.sem = key
            op.cnt = self.dma_cnt[key]
        op.deps = deps
        for t in r:
            self.readers.setdefault(t, []).append(op)
        for t in w:
            self.last_w[t] = op
            self.readers[t] = []
        self.ops[eng].append(op)
        return op

    def all_tails(self):
        tails = []
        for e in ENGS:
            for op in reversed(self.ops[e]):
                if not op.is_dma:
                    tails.append(op)
                    break
        tails.extend(self.dma_last.values())
        return tails

    def setup(self, st):
        nc = self.nc
        self.esem = {e: st.enter_context(nc.semaphore("s_" + e)) for e in ENGS}
        self.dsem = {}
        for e in ("sp", "pool", "act"):
            for i in range(self.n_dma_sems):
                self.dsem[(e, i)] = st.enter_context(nc.semaphore("d_%s_%d" % (e, i)))
        self.emitted = {e: 0 for e in ENGS}
        self.sigc = {e: 0 for e in ENGS}
        self.waited_e = {e: {x: 0 for x in ENGS} for e in ENGS}
        self.waited_d = {e: {} for e in ENGS}
        self.done_ops = set()

    def flush(self, final=False):
        nc = self.nc
        pend = {e: self.ops[e][self.emitted[e]:] for e in ENGS}
        for e in ENGS:
            for op in pend[e]:
                for d in op.deps:
                    if not d.is_dma and not (d.eng == e and e == "pe") and id(d) not in self.done_ops:
                        d.signal = True
        for e in ENGS:
            for op in pend[e]:
                if (not op.is_dma) and op.signal:
                    self.sigc[e] += 1
                    op.sig_idx = self.sigc[e]
        prog = self
        esem, dsem = self.esem, self.dsem
        with nc.Block() as block:
            def run(e, h):
                if e == "pool":
                    prog.bc_reg = h.to_reg(NE * CAP - 1)
                waited_e = prog.waited_e[e]
                waited_d = prog.waited_d[e]
                for op in pend[e]:
                    need_e = {}
                    need_d = {}
                    for d in op.deps:
                        if d.is_dma:
                            if waited_d.get(d.sem, 0) < d.cnt:
                                need_d[d.sem] = max(need_d.get(d.sem, 0), d.cnt)
                        else:
                            if d.eng == e and e == "pe":
                                continue
                            if d.sig_idx == 0:
                                assert id(d) in prog.done_ops, "unsignalled dep in same phase"
                                continue
                            if waited_e[d.eng] < d.sig_idx:
                                need_e[d.eng] = max(need_e.get(d.eng, 0), d.sig_idx)
                    for x, v in need_e.items():
                        h.wait_ge(esem[x], v); waited_e[x] = v
                    for k, v in need_d.items():
                        h.wait_ge(dsem[k], v); waited_d[k] = v
                    ins = op.fn(h)
                    if op.is_dma:
                        ins.then_inc(dsem[op.sem], 16)
                    elif op.signal:
                        ins.then_inc(esem[e], 1)
                if final:
                    for key, cnt in prog.dma_cnt.items():
                        if key[0] == e:
                            h.wait_ge(dsem[key], cnt)

            @block.sync
            def _(h):
                run("sp", h)

            @block.scalar
            def _(h):
                run("act", h)

            @block.vector
            def _(h):
                run("dve", h)

            @block.gpsimd
            def _(h):
                run("pool", h)

            @block.tensor
            def _(h):
                run("pe", h)
        for e in ENGS:
            for op in pend[e]:
                self.done_ops.add(id(op))
            self.emitted[e] = len(self.ops[e])


def build_nc(debug=False, n_prefix=NTILE, n_own=NTILE, n_exp=NE, final=True):
    NEd = max(1, n_exp)
    nc = bass.Bass("TRN2", target_bir_lowering=False)
    P = Prog(nc)
    A = P.add

    def din(name, shape, dt=F32):
        return nc.dram_tensor(name, list(shape), dt, kind="ExternalInput").ap()

    xT = din("xT", [128, KC, 2 * TOWN])
    pT = din("pT", [128, 2, TOWN])
    cc = din("cc", [128, 516])
    cbf_d = din("cbf", [128, NCB])
    cf_d = din("cf", [128, NCF])
    win = din("win", [NWIN, 128, KC, 128])
    wv_d = din("wv", [128, KC, 256])
    wproj = din("wproj", [4, KC, 128, KC, 128])
    wrg_d = din("wrg", [8, 128, 2, 2, 256])
    wrouter_d = din("wrouter", [128, KC, NE])
    wple_d = din("wple", [128, 2, D])
    w1_d = din("w1", [NEd, 32, 128, KC, 128])
    w2_d = din("w2", [NEd, 4, 128, KC, 512])
    b2_d = din("b2", [NEd, D])
    outT = nc.dram_tensor("outT", [128, KC, TOWN], F32, kind="ExternalOutput").ap()
    skind = "ExternalOutput" if debug else "Internal"
    hmid = nc.dram_tensor("hmid", [128, KC, TOWN], F32, kind=skind).ap()
    xg = nc.dram_tensor("xg", [NE * CAP, D], BF16, kind="Internal").ap()
    yg = nc.dram_tensor("yg", [NE * CAP, D], F32, kind="Internal").ap()

    with contextlib.ExitStack() as gst:
        def sbg(name, shape, dt):
            return gst.enter_context(nc.sbuf_tensor(name, list(shape), dt))

        P.setup(gst)
        banks = [gst.enter_context(nc.psum_tensor("bank%d" % i, [128, 512], F32)) for i in range(8)]
        bank_rr = [0]

        def nb():
            i = bank_rr[0] % 8
            bank_rr[0] += 1
            return banks[i], ("bank", i)

        cbf = sbg("cbf_t", [128, NCB], BF16)
        cf = sbg("cf_t", [128, NCF], F32)
        mb40 = sbg("mb40", [128, 512], BF16)
        flags = sbg("flags", [128, 4], F32)
        wbufs = [sbg("wbuf%d" % i, [128, KC, 128], BF16) for i in range(4)]
        wrr = [0]
        dest_f = sbg("dest_f", [128, 64], F32)
        dest_i = sbg("dest_i", [128, 64], I32)
        w4 = sbg("w4", [128, 64], F32)
        bsc = sbg("bsc", [128, 8], F32)

        ident_bf = cbf[:, B_ID:B_ID + 128]
        lt_bf = cbf[:, B_LT:B_LT + 128]
        ones_bf = cbf[:, B_ONES:B_ONES + 128]
        mb4 = cbf[:, B_MB4:B_MB4 + 512]
        oneslo = cbf[:, B_OLO:B_OLO + 128]
        oneshi = cbf[:, B_OHI:B_OHI + 128]
        ident_f = cf[:, O_IDF:O_IDF + 128]

        A("pool", lambda h: h.dma_start(out=cbf[:], in_=cbf_d), w=["cbf"], dma=True)
        A("pool", lambda h: h.dma_start(out=mb40[:], in_=cc[:, 0:512]), w=["mb40"], dma=True)
        A("sp", lambda h: h.dma_start(out=cf[:], in_=cf_d), w=["cf"], dma=True)
        A("sp", lambda h: h.dma_start(out=flags[:], in_=cc[:, 512:516]), w=["flags"], dma=True)

        def wstream(src):
            i = wrr[0] % len(wbufs)
            wrr[0] += 1
            t = wbufs[i]
            A("pool", lambda h: h.dma_start(out=t[:], in_=src), w=[("wb", i)], dma=True)
            return t, ("wb", i)

        bar_n = [0]

        def barrier():
            tails = P.all_tails()
            n = bar_n[0]
            bar_n[0] += 1
            for e in ("act", "dve", "pool"):
                col = {"act": 0, "dve": 1, "pool": 2}[e]
                if e == "act":
                    A(e, lambda h, col=col: h.memzero(bsc[:, col:col + 1]), w=[("bsc", e)], extra=tails)
                else:
                    A(e, lambda h, col=col: h.memset(bsc[:, col:col + 1], 0.0), w=[("bsc", e)], extra=tails)
            b, bt = nb()
            A("pe", lambda h: h.matmul(b[0:1, 0:2], lhsT=ones_bf[0:1, 0:1], rhs=ones_bf[0:1, 0:2], start=True, stop=True),
              r=["cbf"], w=[bt], extra=tails)
            A("sp", lambda h: h.dma_start(out=bsc[:, 4:8], in_=cc[:, 512:516]), w=[("bsc", "sp")], dma=True, extra=tails)

        with contextlib.ExitStack() as st:
            def sb(name, shape, dt):
                return st.enter_context(nc.sbuf_tensor(name, list(shape), dt))

            hT = sb("hT", [128, KC, NT], F32)
            xn = sb("xn", [128, KC, NT], BF16)
            qT = sb("qT", [128, KC, NT], BF16)
            kT = sb("kT", [128, 4, NT + 128], BF16)
            vlo = sb("vlo", [128, 5, 4, 128], BF16)
            vhi = sb("vhi", [128, 5, 4, 128], BF16)
            yat = sb("yat", [128, KC, NT], BF16)
            ylr = sb("ylr", [128, KC, NT], BF16)
            wv = sb("wv_t", [128, KC, 256], BF16)
            wrgt = [sb("wrgt%d" % i, [128, 2, 2, 256], BF16) for i in range(2)]
            wrt = sb("wrt", [128, KC, NE], BF16)
            sqb = [sb("sqb%d" % i, [128, NT], BF16) for i in range(2)]
            rs = sb("rs", [128, NT], F32)
            rstd = sb("rstd", [128, NT], F32)
            u = sb("u", [128, 2, NT + 3], F32)
            uc = sb("uc", [128, 2, NT], F32)
            ucb = sb("ucb", [128, 2, NT], BF16)
            uh = sb("uh", [128, KC, 3], F32)
            hstate = sb("hstate", [128, KC], F32)
            rt = [sb("rt%d" % i, [128, NT], F32) for i in range(2)]
            igt = [sb("igt%d" % i, [128, NT], F32) for i in range(2)]
            at = [sb("at%d" % i, [128, NT], F32) for i in range(2)]
            a2t = [sb("a2t%d" % i, [128, NT], F32) for i in range(2)]
            hseq = [sb("hseq%d" % i, [128, NT], F32) for i in range(2)]
            gt = [sb("gt%d" % i, [128, NT], F32) for i in range(2)]
            t1 = [sb("t1%d" % i, [128, NT], F32) for i in range(2)]
            pm = [sb("pm%d" % i, [128, 512], BF16) for i in range(3)]
            dent = [sb("dent%d" % i, [128, 128], F32) for i in range(2)]
            rect = [sb("rect%d" % i, [128, 128], F32) for i in range(2)]
            nsp8 = sb("nsp8", [128, KC], F32)
            nsp16 = sb("nsp16", [128, KC], F32)
            esink = sb("esink", [128, KC], F32)
            hbrg = sb("hbrg", [128, 32], F32)
            hbin = sb("hbin", [128, NWIN], F32)
            xtok = sb("xtok", [128, D], BF16)
            lg = sb("lg", [128, NE], F32)
            top8 = sb("top8", [128, 8], F32)
            ntop = sb("ntop", [128, 1], F32)
            mk = sb("mk", [128, NE], F32)
            mkb = sb("mkb", [128, NE], BF16)
            rk = sb("rk", [128, NE], F32)
            okr = sb("okr", [128, NE], F32)
            cntb = sb("cntb", [128, NE], F32)
            ex4 = sb("ex4", [128, 4], F32)
            s4 = sb("s4", [128, 1], F32)
            junk = sb("junk", [128, NE], F32)

            A("pool", lambda h: h.dma_start(out=wv[:], in_=wv_d), w=["wv"], dma=True)
            A("pool", lambda h: h.dma_start(out=wrt[:], in_=wrouter_d), w=["wrt"], dma=True)
            A("dve", lambda h: h.memset(vlo[:], 0.0), w=["vlo"])
            A("dve", lambda h: h.memset(vhi[:], 0.0), w=["vhi"])
            A("dve", lambda h: h.memset(kT[:], 0.0), w=["kT"])
            A("dve", lambda h: h.memset(uh[:], 0.0), w=["uh"])
            A("dve", lambda h: h.memset(hstate[:], 0.0), w=["hstate"])
            A("dve", lambda h: h.memset(cntb[:], 0.0), w=["cntb"])
            A("act", lambda h: h.activation(out=nsp8[:], in_=cf[:, O_LAM:O_LAM + 16], func=AF.Exp, scale=-1.0),
              r=["cf"], w=["nsp8"])
            A("act", lambda h: h.activation(out=nsp8[:], in_=nsp8[:], func=AF.Ln, bias=1.0), r=["nsp8"], w=["nsp8"])
            A("dve", lambda h: h.tensor_scalar(out=nsp16[:], in0=nsp8[:], scalar1=-4.0, scalar2=None, op0=ALU.mult),
              r=["nsp8"], w=["nsp16"])
            A("dve", lambda h: h.tensor_scalar(out=hbrg[:], in0=cf[:, O_BRG:O_BRG + 32], scalar1=0.5, scalar2=None, op0=ALU.mult),
              r=["cf"], w=["hbrg"])
            A("dve", lambda h: h.tensor_scalar(out=hbin[:], in0=cf[:, O_BIN:O_BIN + NWIN], scalar1=0.5, scalar2=None, op0=ALU.mult),
              r=["cf"], w=["hbin"])
            A("dve", lambda h: h.tensor_scalar(out=nsp8[:], in0=nsp8[:], scalar1=-8.0, scalar2=None, op0=ALU.mult),
              r=["nsp8", "nsp16"], w=["nsp8"])
            A("act", lambda h: h.activation(out=esink[:], in_=cf[:, O_SINK:O_SINK + 16], func=AF.Exp),
              r=["cf"], w=["esink"])

            def rmsnorm_to_xn(gidx, dst, dst_tok):
                b, bt = nb()
                for k in range(KC):
                    sq = sqb[k % 2]
                    A("act", lambda h, k=k, sq=sq: h.activation(out=sq[:], in_=hT[:, k, :], func=AF.Square),
                      r=[("h", k)], w=[("sq", k % 2)])
                    A("pe", lambda h, k=k, sq=sq: h.matmul(b[:, :], lhsT=ones_bf, rhs=sq[:], start=(k == 0), stop=(k == KC - 1)),
                      r=[("sq", k % 2), "cbf"], w=[bt])
                A("act", lambda h: h.activation(out=rs[:], in_=b[:, :], func=AF.Sqrt, scale=1.0 / D, bias=EPS),
                  r=[bt], w=["rs"])
                A("dve", lambda h: h.reciprocal(out=rstd[:], in_=rs[:]), r=["rs"], w=["rstd"])
                for k in range(KC):
                    A("dve", lambda h, k=k: h.scalar_tensor_tensor(
                        out=dst[:, k, :], in0=hT[:, k, :], scalar=cf[:, O_GAIN + gidx * 16 + k:O_GAIN + gidx * 16 + k + 1],
                        in1=rstd[:], op0=ALU.mult, op1=ALU.mult),
                      r=[("h", k), "rstd", "cf"], w=[(dst_tok, k)])

            def win_chunk(cidx):
                wt, wtok = wstream(win[cidx])
                b, bt = nb()
                for k in range(KC):
                    A("pe", lambda h, k=k: h.matmul(b[:, :], lhsT=wt[:, k, :], rhs=xn[:, k, :], start=(k == 0), stop=(k == KC - 1)),
                      r=[wtok, ("xn", k)], w=[bt])
                return b, bt

            def bias_col(cidx):
                return cf[:, O_BIN + cidx:O_BIN + cidx + 1]

            def kv_chunks():
                for kvh in range(4):
                    b, bt = win_chunk(C_K + kvh)
                    A("act", lambda h, b=b, kvh=kvh: h.activation(out=kT[:, kvh, 128:128 + NT], in_=b[:, :], func=AF.Identity,
                                                                  bias=bias_col(C_K + kvh)),
                      r=[bt, "cf"], w=["kT"])
                for half in range(2):
                    b, bt = nb()
                    for tb2 in range(2):
                        tb = half * 2 + tb2
                        for k in range(KC):
                            A("pe", lambda h, k=k, tb=tb, tb2=tb2, b=b: h.matmul(
                                b[:, tb2 * 256:(tb2 + 1) * 256], lhsT=xn[:, k, tb * 128:(tb + 1) * 128], rhs=wv[:, k, :],
                                start=(k == 0), stop=(k == KC - 1)),
                              r=[("xn", k), "wv"], w=[bt])
                    for tb2 in range(2):
                        tb = half * 2 + tb2
                        src = b[:, tb2 * 256:(tb2 + 1) * 256].rearrange("p (a c) -> p a c", c=64)
                        bvv = cf[:, O_BV:O_BV + 256].rearrange("p (a c) -> p a c", c=64)
                        A("dve", lambda h, tb=tb, src=src, bvv=bvv: h.tensor_tensor(out=vlo[:, 1 + tb, :, 0:64], in0=src, in1=bvv, op=ALU.add),
                          r=[bt, "cf"], w=["vlo"])
                        A("dve", lambda h, tb=tb, src=src, bvv=bvv: h.tensor_tensor(out=vhi[:, 1 + tb, :, 64:128], in0=src, in1=bvv, op=ALU.add),
                          r=[bt, "cf"], w=["vhi"])

            def kv_halo():
                A("act", lambda h: h.activation(out=kT[:, :, 0:128], in_=kT[:, :, NT:NT + 128], func=AF.Copy), r=["kT"], w=["kT"])
                A("dve", lambda h: h.tensor_copy(out=vlo[:, 0, :, :], in_=vlo[:, 4, :, :]), r=["vlo"], w=["vlo"])
                A("dve", lambda h: h.tensor_copy(out=vhi[:, 0, :, :], in_=vhi[:, 4, :, :]), r=["vhi"], w=["vhi"])

            def lru_block(n, prefix, first_prefix, first_own):
                alt = prefix and (n % 2 == 1)
                if alt:
                    ucL = [gt[0][:], gt[1][:]]
                    ucbL = [pm[0][:], pm[1][:]]
                    uct = [("gt", 0), ("gt", 1)]
                    ucbt = [("pm", 0), ("pm", 1)]
                else:
                    ucL = [uc[:, 0, :], uc[:, 1, :]]
                    ucbL = [ucb[:, 0, :], ucb[:, 1, :]]
                    uct = [("uc", 0), ("uc", 1)]
                    ucbt = [("ucb", 0), ("ucb", 1)]
                wg = wrgt[n % 2]
                A("pool", lambda h: h.dma_start(out=wg[:], in_=wrg_d[n]), w=[("wrg", n % 2)], dma=True)
                for j in range(2):
                    ci = C_LRU + 4 * n + j
                    b, bt = win_chunk(ci)
                    A("act", lambda h, b=b, j=j, ci=ci: h.activation(out=u[:, j, 3:3 + NT], in_=b[:, :], func=AF.Identity, bias=bias_col(ci)),
                      r=[bt, "cf"], w=[("u", j)])
                A("dve", lambda h: h.tensor_copy(out=u[:, :, 0:3], in_=uh[:, 2 * n:2 * n + 2, :]), r=["uh"], w=[("u", 0), ("u", 1)])
                A("dve", lambda h: h.tensor_copy(out=uh[:, 2 * n:2 * n + 2, :], in_=u[:, :, NT:NT + 3]), r=[("u", 0), ("u", 1)], w=["uh"])
                for j in range(2):
                    c = 2 * n + j
                    A("act", lambda h, j=j, c=c: h.activation(out=ucL[j], in_=u[:, j, 0:NT], func=AF.Identity,
                                                             scale=cf[:, O_CONVW + c:O_CONVW + c + 1], bias=cf[:, O_CONVB + c:O_CONVB + c + 1]),
                      r=[("u", j), "cf"], w=[uct[j]])
                for tap in range(1, 4):
                    for j in range(2):
                        c = 2 * n + j
                        A("dve", lambda h, j=j, c=c, tap=tap: h.scalar_tensor_tensor(
                            out=ucL[j], in0=u[:, j, tap:tap + NT], scalar=cf[:, O_CONVW + tap * 16 + c:O_CONVW + tap * 16 + c + 1],
                            in1=ucL[j], op0=ALU.mult, op1=ALU.add),
                          r=[("u", j), uct[j], "cf"], w=[uct[j]])
                for j in range(2):
                    A("act", lambda h, j=j: h.activation(out=ucbL[j], in_=ucL[j], func=AF.Copy), r=[uct[j]], w=[ucbt[j]])
                yield
                gb_ = {}
                for j in range(2):
                    for ax in range(2):
                        bb, btt = nb()
                        gb_[(j, ax)] = (bb, btt)
                        for i in range(2):
                            A("pe", lambda h, ax=ax, bb=bb, i=i, j=j: h.matmul(
                                bb[:, :], lhsT=wg[:, ax, i, j * 128:(j + 1) * 128], rhs=ucbL[i], start=(i == 0), stop=(i == 1)),
                              r=[("wrg", n % 2), ucbt[i]], w=[btt])
                for j in range(2):
                    c = 2 * n + j
                    br, btr = gb_[(j, 0)]
                    bx, btx = gb_[(j, 1)]
                    A("act", lambda h, br=br, c=c, j=j: h.activation(out=rt[j][:], in_=br[:, :], func=AF.Tanh, scale=0.5, bias=hbrg[:, c:c + 1]),
                      r=[btr, "hbrg"], w=[("rt", j)])
                    A("act", lambda h, bx=bx, c=c, j=j: h.activation(out=igt[j][:], in_=bx[:, :], func=AF.Tanh, scale=0.5, bias=hbrg[:, 16 + c:16 + c + 1]),
                      r=[btx, "hbrg"], w=[("igt", j)])
                for j in range(2):
                    c = 2 * n + j
                    A("act", lambda h, c=c, j=j: h.activation(out=at[j][:], in_=rt[j][:], func=AF.Exp, scale=nsp16[:, c:c + 1], bias=nsp16[:, c:c + 1]),
                      r=[("rt", j), "nsp16"], w=[("at", j)])
                    A("act", lambda h, c=c, j=j: h.activation(out=a2t[j][:], in_=rt[j][:], func=AF.Exp, scale=nsp8[:, c:c + 1], bias=nsp8[:, c:c + 1]),
                      r=[("rt", j), "nsp8"], w=[("a2t", j)])
                for j in range(2):
                    A("act", lambda h, j=j: h.activation(out=a2t[j][:], in_=a2t[j][:], func=AF.Sqrt, scale=-1.0, bias=1.0), r=[("a2t", j)], w=[("a2t", j)])
                for j in range(2):
                    if first_prefix:
                        A("dve", lambda h, j=j: h.memset(a2t[j][:, 0:1], 1.0), r=[("a2t", j)], w=[("a2t", j)])
                    if first_own:
                        A("dve", lambda h, j=j: h.tensor_scalar(out=a2t[j][:, 0:1], in0=a2t[j][:, 0:1], scalar1=flags[:, 1:2], scalar2=flags[:, 2:3],
                                                                op0=ALU.mult, op1=ALU.add), r=[("a2t", j), "flags"], w=[("a2t", j)])
                for j in range(2):
                    A("dve", lambda h, j=j: h.scalar_tensor_tensor(out=igt[j][:], in0=igt[j][:], scalar=1.0, in1=ucL[j], op0=ALU.add, op1=ALU.mult),
                      r=[("igt", j), uct[j]], w=[("igt", j)])
                for j in range(2):
                    A("dve", lambda h, j=j: h.scalar_tensor_tensor(out=igt[j][:], in0=igt[j][:], scalar=0.5, in1=a2t[j][:], op0=ALU.mult, op1=ALU.mult),
                      r=[("igt", j), ("a2t", j)], w=[("igt", j)])
                for j in range(2):
                    c = 2 * n + j
                    A("dve", lambda h, c=c, j=j: h.tensor_tensor_scan(out=hseq[j][:], data0=at[j][:], data1=igt[j][:], initial=hstate[:, c:c + 1],
                                                                      op0=ALU.mult, op1=ALU.add), r=[("at", j), ("igt", j), "hstate"], w=[("hseq", j)])
                for j in range(2):
                    c = 2 * n + j
                    A("dve", lambda h, c=c, j=j: h.tensor_copy(out=hstate[:, c:c + 1], in_=hseq[j][:, NT - 1:NT]), r=[("hseq", j)], w=["hstate"])
                yield
                if not prefix:
                    for j in range(2):
                        ci = C_LRU + 4 * n + 2 + j
                        b, bt = win_chunk(ci)
                        A("act", lambda h, b=b, ci=ci, j=j: h.activation(out=gt[j][:], in_=b[:, :], func=AF.Identity, bias=bias_col(ci)),
                          r=[bt, "cf"], w=[("gt", j)])
                        A("act", lambda h, j=j: h.activation(out=t1[j][:], in_=gt[j][:], func=AF.Square), r=[("gt", j)], w=[("t1", j)])
                    for j in range(2):
                        A("dve", lambda h, j=j: h.tensor_scalar(out=t1[j][:], in0=t1[j][:], scalar1=0.044715, scalar2=1.0, op0=ALU.mult, op1=ALU.add),
                          r=[("t1", j)], w=[("t1", j)])
                    for j in range(2):
                        A("dve", lambda h, j=j: h.tensor_tensor(out=t1[j][:], in0=t1[j][:], in1=gt[j][:], op=ALU.mult), r=[("t1", j), ("gt", j)], w=[("t1", j)])
                    for j in range(2):
                        A("act", lambda h, j=j: h.activation(out=t1[j][:], in_=t1[j][:], func=AF.Tanh, scale=0.7978845608028654), r=[("t1", j)], w=[("t1", j)])
                    for j in range(2):
                        A("dve", lambda h, j=j: h.scalar_tensor_tensor(out=t1[j][:], in0=t1[j][:], scalar=1.0, in1=gt[j][:], op0=ALU.add, op1=ALU.mult),
                          r=[("t1", j), ("gt", j)], w=[("t1", j)])
                    for j in range(2):
                        c = 2 * n + j
                        A("dve", lambda h, c=c, j=j: h.scalar_tensor_tensor(out=ylr[:, c, :], in0=t1[j][:], scalar=0.5, in1=hseq[j][:], op0=ALU.mult, op1=ALU.mult),
                          r=[("t1", j), ("hseq", j)], w=[("ylr", c)])
                    yield

            def attention(first_own):
                iters = [(qb, c) for qb in range(4) for c in range(KC)]
                sbanks = {}

                def s_stage(i):
                    qb, c = iters[i]
                    kvh = c // 4
                    p = pm[i % 3]
                    use0 = first_own and qb == 0
                    for hh in range(2):
                        lo, hi = hh * 64, hh * 64 + 64
                        b, bt = nb()
                        if use0:
                            A("pe", lambda h, b=b: h.matmul(b[:, 0:256], lhsT=ident_bf, rhs=mb40[:, 0:256], start=True, stop=False),
                              r=["cbf", "mb40"], w=[bt])
                        else:
                            A("pe", lambda h, b=b: h.matmul(b[:, 0:256], lhsT=ident_bf, rhs=mb4[:, 0:256], start=True, stop=False),
                              r=["cbf"], w=[bt])
                        for pc in range(2):
                            col = pc * 128
                            kb = qb + pc
                            A("pe", lambda h, b=b, lo=lo, hi=hi, col=col, kb=kb, pc=pc: h.matmul(
                                b[:, col:col + 128], lhsT=kT[lo:hi, kvh, kb * 128:(kb + 1) * 128], rhs=qT[lo:hi, c, qb * 128:(qb + 1) * 128],
                                start=False, stop=(pc == 1)),
                              r=["kT", ("q", c)], w=[bt])
                        A("act", lambda h, b=b, hh=hh: h.activation(out=p[:, hh * 256:(hh + 1) * 256], in_=b[:, 0:256], func=AF.Exp, scale=0.125),
                          r=[bt], w=[("pm", i % 3)])

                def pv_stage(i):
                    qb, c = iters[i]
                    kvh = c // 4
                    p = pm[i % 3]
                    b, bt = nb()
                    seq = [(vlo, qb, 0), (vlo, qb + 1, 1), (vhi, qb, 2), (vhi, qb + 1, 3)]
                    for n_, (vt, blk, pc) in enumerate(seq):
                        A("pe", lambda h, vt=vt, blk=blk, pc=pc, n_=n_: h.matmul(
                            b[:, 0:128], lhsT=vt[:, blk, kvh, :], rhs=p[:, pc * 128:(pc + 1) * 128], start=(n_ == 0), stop=(n_ == 3)),
                          r=["vlo", "vhi", ("pm", i % 3)], w=[bt])
                    seq2 = [(oneslo, 0), (oneslo, 1), (oneshi, 2), (oneshi, 3)]
                    for n_, (ot, pc) in enumerate(seq2):
                        A("pe", lambda h, ot=ot, pc=pc, n_=n_: h.matmul(
                            b[:, 128:256], lhsT=ot, rhs=p[:, pc * 128:(pc + 1) * 128], start=(n_ == 0), stop=(n_ == 3)),
                          r=["cbf", ("pm", i % 3)], w=[bt])
                    de = dent[i % 2]
                    re = rect[i % 2]
                    A("dve", lambda h: h.tensor_scalar(out=de[:], in0=b[:, 128:256], scalar1=esink[:, c:c + 1], scalar2=None, op0=ALU.add),
                      r=[bt, "esink"], w=[("dent", i % 2)])
                    A("dve", lambda h: h.reciprocal(out=re[:], in_=de[:]), r=[("dent", i % 2)], w=[("rect", i % 2)])
                    A("dve", lambda h: h.tensor_tensor(out=yat[:, c, qb * 128:(qb + 1) * 128], in0=b[:, 0:128], in1=re[:], op=ALU.mult),
                      r=[bt, ("rect", i % 2)], w=[("yat", c)])

                n_it = len(iters)
                s_stage(0)
                s_stage(1)
                for i in range(n_it):
                    pv_stage(i)
                    if i + 2 < n_it:
                        s_stage(i + 2)
                    if i % 2 == 1:
                        yield

            def merge_and_out(ti):
                mg = qT
                for j in range(KC):
                    jj = j % 2
                    ga, gl, m1, m2 = rt[jj], igt[jj], at[jj], a2t[jj]
                    wa, wat = wstream(wproj[0, j])
                    ba, bat = nb()
                    for k in range(KC):
                        A("pe", lambda h, k=k, wa=wa, ba=ba: h.matmul(ba[:, :], lhsT=wa[:, k, :], rhs=yat[:, k, :], start=(k == 0), stop=(k == KC - 1)),
                          r=[wat, ("yat", k)], w=[bat])
                    wb_, wbt = wstream(wproj[1, j])
                    bb, bbt = nb()
                    for k in range(KC):
                        A("pe", lambda h, k=k, wb_=wb_, bb=bb: h.matmul(bb[:, :], lhsT=wb_[:, k, :], rhs=ylr[:, k, :], start=(k == 0), stop=(k == KC - 1)),
                          r=[wbt, ("ylr", k)], w=[bbt])
                    cga = C_GATE + 2 * j
                    bga, bgat = win_chunk(cga)
                    bgl, bglt = win_chunk(cga + 1)
                    A("act", lambda h, bga=bga, cga=cga, ga=ga: h.activation(out=ga[:], in_=bga[:, :], func=AF.Tanh, scale=0.5, bias=hbin[:, cga:cga + 1]),
                      r=[bgat, "hbin"], w=[("rt", jj)])
                    A("act", lambda h, bgl=bgl, cga=cga, gl=gl: h.activation(out=gl[:], in_=bgl[:, :], func=AF.Tanh, scale=0.5, bias=hbin[:, cga + 1:cga + 2]),
                      r=[bglt, "hbin"], w=[("igt", jj)])
                    A("dve", lambda h, ba=ba, ga=ga, m1=m1: h.scalar_tensor_tensor(out=m1[:], in0=ga[:], scalar=1.0, in1=ba[:, :], op0=ALU.add, op1=ALU.mult),
                      r=[bat, ("rt", jj)], w=[("at", jj)])
                    A("dve", lambda h, bb=bb, gl=gl, m2=m2: h.scalar_tensor_tensor(out=m2[:], in0=gl[:], scalar=1.0, in1=bb[:, :], op0=ALU.add, op1=ALU.mult),
                      r=[bbt, ("igt", jj)], w=[("a2t", jj)])
                    A("dve", lambda h, j=j, m1=m1, m2=m2: h.tensor_tensor(out=mg[:, j, :], in0=m1[:], in1=m2[:], op=ALU.add),
                      r=[("at", jj), ("a2t", jj)], w=[("q", j)])
                for j in range(KC):
                    wo, wot = wstream(wproj[2, j])
                    bo, bot = nb()
                    for k in range(KC):
                        A("pe", lambda h, k=k, wo=wo, bo=bo: h.matmul(bo[:, :], lhsT=wo[:, k, :], rhs=mg[:, k, :], start=(k == 0), stop=(k == KC - 1)),
                          r=[wot, ("q", k)], w=[bot])
                    A("dve", lambda h, j=j, bo=bo: h.scalar_tensor_tensor(out=hT[:, j, :], in0=bo[:, :], scalar=0.5, in1=hT[:, j, :], op0=ALU.mult, op1=ALU.add),
                      r=[bot, ("h", j)], w=[("h", j)])
                for half in range(2):
                    A("sp", lambda h, half=half: h.dma_start(out=hmid[:, half * 8:(half + 1) * 8, ti * NT:(ti + 1) * NT],
                                                            in_=hT[:, half * 8:(half + 1) * 8, :]),
                      r=[("h", k) for k in range(half * 8, half * 8 + 8)], w=[("hmid", ti, half)], dma=True)

            def route_and_dispatch(ti):
                rmsnorm_to_xn(1, xn, "xn")
                for tb in range(4):
                    blk = ti * 4 + tb
                    b, bt = nb()
                    for k in range(KC):
                        A("pe", lambda h, k=k, tb=tb, b=b: h.matmul(b[:, 0:NE], lhsT=xn[:, k, tb * 128:(tb + 1) * 128], rhs=wrt[:, k, :],
                                                                    start=(k == 0), stop=(k == KC - 1)),
                          r=[("xn", k), "wrt"], w=[bt])
                    A("dve", lambda h, b=b: h.tensor_tensor(out=lg[:], in0=b[:, 0:NE], in1=cf[:, O_BROUT:O_BROUT + NE], op=ALU.add),
                      r=[bt, "cf"], w=["lg"])
                    A("dve", lambda h: h.max(out=top8[:], in_=lg[:]), r=["lg"], w=["top8"])
                    A("dve", lambda h: h.tensor_scalar(out=ntop[:], in0=top8[:, 0:1], scalar1=-1.0, scalar2=None, op0=ALU.mult),
                      r=["top8"], w=["ntop"])
                    A("dve", lambda h: h.tensor_scalar(out=mk[:], in0=lg[:], scalar1=top8[:, 3:4], scalar2=None, op0=ALU.is_ge),
                      r=["lg", "top8"], w=["mk"])
                    A("dve", lambda h: h.tensor_copy(out=mkb[:], in_=mk[:]), r=["mk"], w=["mkb"])
                    br_, brt = nb()
                    A("pe", lambda h, br_=br_: h.matmul(br_[:, 0:NE], lhsT=lt_bf, rhs=mkb[:], start=True, stop=True), r=["cbf", "mkb"], w=[brt])
                    A("pe", lambda h, br_=br_: h.matmul(br_[:, 64:64 + NE], lhsT=ones_bf, rhs=mkb[:], start=True, stop=True), r=["cbf", "mkb"], w=[brt])
                    A("dve", lambda h, br_=br_: h.tensor_tensor(out=rk[:], in0=br_[:, 0:NE], in1=cntb[:], op=ALU.add), r=[brt, "cntb"], w=["rk"])
                    A("dve", lambda h, br_=br_: h.tensor_tensor(out=cntb[:], in0=br_[:, 64:64 + NE], in1=cntb[:], op=ALU.add), r=[brt, "cntb", "rk"], w=["cntb"])
                    A("dve", lambda h: h.tensor_scalar(out=okr[:], in0=rk[:], scalar1=float(CAP), scalar2=None, op0=ALU.is_lt), r=["rk"], w=["okr"])
                    A("dve", lambda h: h.tensor_tensor(out=okr[:], in0=okr[:], in1=mk[:], op=ALU.mult), r=["okr", "mk"], w=["okr"])
                    A("dve", lambda h: h.tensor_tensor(out=rk[:], in0=rk[:], in1=cf[:, O_ECAP:O_ECAP + NE], op=ALU.add), r=["rk", "cf"], w=["rk"])
                    A("dve", lambda h: h.tensor_tensor(out=rk[:], in0=rk[:], in1=okr[:], op=ALU.mult), r=["rk", "okr"], w=["rk"])
                    A("dve", lambda h: h.tensor_scalar(out=rk[:], in0=rk[:], scalar1=BIG, scalar2=None, op0=ALU.add), r=["rk"], w=["rk"])
                    for kk in range(4):
                        A("dve", lambda h, kk=kk, blk=blk: h.scalar_tensor_tensor(
                            out=junk[:], in0=lg[:], scalar=top8[:, kk:kk + 1], in1=rk[:], op0=ALU.is_equal, op1=ALU.mult,
                            accum_out=dest_f[:, blk * 4 + kk:blk * 4 + kk + 1]),
                          r=["lg", "top8", "rk", "junk"], w=["junk", ("dest_f", blk)])
                    A("dve", lambda h, blk=blk: h.tensor_copy(out=dest_i[:, blk * 4:blk * 4 + 4], in_=dest_f[:, blk * 4:blk * 4 + 4]),
                      r=[("dest_f", blk)], w=[("dest_i", blk)])
                    A("act", lambda h: h.activation(out=ex4[:], in_=top8[:, 0:4], func=AF.Exp, bias=ntop[:, 0:1], accum_out=s4[:]),
                      r=["top8", "ntop"], w=["ex4", "s4"])
                    A("dve", lambda h: h.reciprocal(out=s4[:], in_=s4[:]), r=["s4"], w=["s4"])
                    A("dve", lambda h, blk=blk: h.tensor_scalar(out=w4[:, blk * 4:blk * 4 + 4], in0=ex4[:], scalar1=s4[:, 0:1], scalar2=None, op0=ALU.mult),
                      r=["ex4", "s4"], w=[("w4", blk)])
                    A("dve", lambda h, blk=blk: h.tensor_scalar(out=ex4[:], in0=dest_f[:, blk * 4:blk * 4 + 4], scalar1=BIG, scalar2=None, op0=ALU.is_lt),
                      r=[("dest_f", blk), "ex4"], w=["ex4"])
                    A("dve", lambda h, blk=blk: h.tensor_tensor(out=w4[:, blk * 4:blk * 4 + 4], in0=w4[:, blk * 4:blk * 4 + 4], in1=ex4[:], op=ALU.mult),
                      r=["ex4", ("w4", blk)], w=[("w4", blk)])
                    for half in range(2):
                        bT, bTt = nb()
                        bv_ = bT[:].bitcast(BF16)
                        for kq in range(8):
                            k = half * 8 + kq
                            A("pe", lambda h, k=k, kq=kq, tb=tb, bv_=bv_: h.transpose(out=bv_[:, kq * 128:(kq + 1) * 128],
                                                                                      in_=xn[:, k, tb * 128:(tb + 1) * 128], identity=ident_bf),
                              r=[("xn", k), "cbf"], w=[bTt])
                        A("act", lambda h, half=half, bv_=bv_: h.activation(out=xtok[:, half * 1024:(half + 1) * 1024], in_=bv_[:, 0:1024], func=AF.Copy),
                          r=[bTt], w=["xtok"])
                    for kk in range(4):
                        A("pool", lambda h, kk=kk, blk=blk: h.indirect_dma_start(
                            out=xg, out_offset=bass.IndirectOffsetOnAxis(ap=dest_i[:, blk * 4 + kk:blk * 4 + kk + 1], axis=0),
                            in_=xtok[:, :], in_offset=None, bounds_check=P.bc_reg, oob_is_err=False),
                          r=["xtok", ("dest_i", blk)], w=["xg"], dma=True)

            def load_tile(tok0):
                for half in range(2):
                    A("sp", lambda h, half=half: h.dma_start(out=hT[:, half * 8:(half + 1) * 8, :],
                                                            in_=xT[:, half * 8:(half + 1) * 8, tok0:tok0 + NT]),
                      w=[("h", k) for k in range(half * 8, half * 8 + 8)], dma=True)

            for pt in range(NTILE - n_prefix, NTILE):
                load_tile(pt * NT)
                rmsnorm_to_xn(0, xn, "xn")
                if pt == NTILE - 1:
                    kv_chunks()
                    kv_halo()
                for n in range(8):
                    for _ in lru_block(n, True, pt == 0, False):
                        pass
            A("dve", lambda h: h.tensor_scalar(out=hstate[:], in0=hstate[:], scalar1=flags[:, 0:1], scalar2=None, op0=ALU.mult),
              r=["hstate", "flags"], w=["hstate"])
            A("dve", lambda h: h.tensor_scalar(out=uh[:], in0=uh[:], scalar1=flags[:, 0:1], scalar2=None, op0=ALU.mult),
              r=["uh", "flags"], w=["uh"])
            for ti in range(n_own):
                load_tile(TOWN + ti * NT)
                if STAGE < 1:
                    continue
                rmsnorm_to_xn(0, xn, "xn")
                if STAGE < 2:
                    continue
                for c in range(KC):
                    b, bt = win_chunk(C_Q + c)
                    A("act", lambda h, b=b, c=c: h.activation(out=qT[:, c, :], in_=b[:, :], func=AF.Identity, bias=bias_col(C_Q + c)),
                      r=[bt, "cf"], w=[("q", c)])
                if STAGE < 3:
                    continue
                kv_chunks()
                if STAGE < 4:
                    continue
                def lru_all(ti=ti):
                    for n in range(8):
                        yield from lru_block(n, False, False, ti == 0)
                gens = [attention(ti == 0), lru_all()]
                while gens:
                    for g in list(gens):
                        try:
                            next(g)
                        except StopIteration:
                            gens.remove(g)
                if debug and ti == 0:
                    for nm, tl, rr in (("dbg_yat", yat, [("yat", k) for k in range(KC)]), ("dbg_q", qT, [("q", k) for k in range(KC)]),
                                       ("dbg_k", kT, ["kT"]), ("dbg_vlo", vlo, ["vlo"]), ("dbg_vhi", vhi, ["vhi"])):
                        dd = nc.dram_tensor(nm, list(tl.shape), BF16, kind="ExternalOutput").ap()
                        A("sp", lambda h, dd=dd, tl=tl: h.dma_start(out=dd, in_=tl[:]), r=rr, w=[nm], dma=True)
                kv_halo()
                if STAGE < 6:
                    continue
                merge_and_out(ti)
                if STAGE < 7:
                    continue
                route_and_dispatch(ti)
            barrier()
            P.flush()

        with contextlib.ExitStack() as st:
            def sb(name, shape, dt):
                return st.enter_context(nc.sbuf_tensor(name, list(shape), dt))

            xte = [sb("xte%d" % i, [128, NJT, D], BF16) for i in range(2)]
            xgT = [sb("xgT%d" % i, [128, KC, CAP], BF16) for i in range(2)]
            actT = [sb("actT%d" % i, [128, KC, CAP], BF16) for i in range(2)]
            w2b = [sb("w2b%d" % i, [128, KC, 512], BF16) for i in range(2)]
            ytok = [sb("ytok%d" % i, [128, NJT, D], F32) for i in range(2)]
            b2r = [sb("b2r%d" % i, [1, D], BF16) for i in range(2)]
            glt = [sb("glt%d" % i, [128, CAP], F32) for i in range(2)]
            sgt = [sb("sgt%d" % i, [128, CAP], F32) for i in range(2)]
            lnt = [sb("lnt%d" % i, [128, CAP], F32) for i in range(2)]
            w2rr = [0]

            def moe_prep(e):
                s = e % 2
                xt_ = xte[s]
                xg_ = xgT[s]
                at_ = actT[s]
                yt_ = ytok[s]
                for jt in range(NJT):
                    A("sp", lambda h, jt=jt, xt_=xt_, e=e: h.dma_start(out=xt_[0:JR[jt], jt, :], in_=xg[e * CAP + jt * 128:e * CAP + jt * 128 + JR[jt], :]),
                      r=["xg"], w=[("xte", s, jt)], dma=True)
                A("pool", lambda h, e=e, s=s: h.dma_start(out=b2r[s][:], in_=b2_d[e:e + 1, :]), w=[("b2r", s)], dma=True)
                for jt in range(NJT):
                    for half in range(2):
                        bT, bTt = nb()
                        bv_ = bT[:].bitcast(BF16)
                        for kq in range(8):
                            k = half * 8 + kq
                            A("pe", lambda h, k=k, kq=kq, jt=jt, bv_=bv_, xt_=xt_: h.transpose(
                                out=bv_[:, kq * 128:kq * 128 + JR[jt]], in_=xt_[0:JR[jt], jt, k * 128:(k + 1) * 128],
                                identity=ident_bf[0:JR[jt], 0:JR[jt]]),
                              r=[("xte", s, jt), "cbf"], w=[bTt])
                        A("act", lambda h, half=half, jt=jt, bv_=bv_, xg_=xg_: h.activation(
                            out=xg_[:, half * 8:(half + 1) * 8, jt * 128:jt * 128 + JR[jt]],
                            in_=bv_[:, 0:1024].rearrange("p (a c) -> p a c", c=128)[:, :, 0:JR[jt]], func=AF.Copy),
                          r=[bTt], w=[("xgT", s)])

            def moe_stage1(e):
                s = e % 2
                xt_ = xte[s]
                xg_ = xgT[s]
                at_ = actT[s]
                yt_ = ytok[s]
                for i in range(KC):
                    bks = []
                    for fc in (i, KC + i):
                        wt, wtok = wstream(w1_d[e, fc])
                        b, bt = nb()
                        for k in range(KC):
                            A("pe", lambda h, k=k, wt=wt, b=b, xg_=xg_: h.matmul(b[:, 0:CAP], lhsT=wt[:, k, :], rhs=xg_[:, k, :],
                                                                                start=(k == 0), stop=(k == KC - 1)),
                              r=[wtok, ("xgT", s)], w=[bt])
                        bks.append((b, bt))
                    (bg, bgt), (bl, blt) = bks
                    g_, s_, l_ = glt[i % 2], sgt[i % 2], lnt[i % 2]
                    cg = O_B1 + e * 32 + i
                    cl = O_B1 + e * 32 + KC + i
                    A("dve", lambda h, bg=bg, g_=g_, cg=cg: h.tensor_scalar(out=g_[:], in0=bg[:, 0:CAP], scalar1=cf[:, cg:cg + 1], scalar2=7.0,
                                                                            op0=ALU.add, op1=ALU.min),
                      r=[bgt, "cf"], w=[("glt", i % 2)])
                    A("act", lambda h, g_=g_, s_=s_: h.activation(out=s_[:], in_=g_[:], func=AF.Sigmoid, scale=1.702),
                      r=[("glt", i % 2)], w=[("sgt", i % 2)])
                    A("dve", lambda h, bl=bl, l_=l_, cl=cl: h.tensor_scalar(out=l_[:], in0=bl[:, 0:CAP], scalar1=cf[:, cl:cl + 1], scalar2=7.0,
                                                                            op0=ALU.add, op1=ALU.min),
                      r=[blt, "cf"], w=[("lnt", i % 2)])
                    A("dve", lambda h, l_=l_: h.tensor_scalar(out=l_[:], in0=l_[:], scalar1=-7.0, scalar2=1.0, op0=ALU.max, op1=ALU.add),
                      r=[("lnt", i % 2)], w=[("lnt", i % 2)])
                    A("dve", lambda h, g_=g_, s_=s_: h.tensor_tensor(out=g_[:], in0=g_[:], in1=s_[:], op=ALU.mult),
                      r=[("glt", i % 2), ("sgt", i % 2)], w=[("glt", i % 2)])
                    A("dve", lambda h, g_=g_, l_=l_, i=i, at_=at_: h.tensor_tensor(out=at_[:, i, :], in0=g_[:], in1=l_[:], op=ALU.mult),
                      r=[("glt", i % 2), ("lnt", i % 2)], w=[("actT", s, i)])

            def moe_stage2(e):
                s = e % 2
                xt_ = xte[s]
                xg_ = xgT[s]
                at_ = actT[s]
                yt_ = ytok[s]
                for dg in range(4):
                    ws = w2rr[0] % 2
                    w2rr[0] += 1
                    w2t = w2b[ws]
                    A("pool", lambda h, w2t=w2t, e=e, dg=dg: h.dma_start(out=w2t[:], in_=w2_d[e, dg], max_dma_last_dim=4096),
                      w=[("w2b", ws)], dma=True)
                    for jt in range(NJT):
                        b, bt = nb()
                        for k in range(KC):
                            A("pe", lambda h, k=k, jt=jt, b=b, w2t=w2t, at_=at_: h.matmul(
                                b[0:JR[jt], :], lhsT=at_[:, k, jt * 128:jt * 128 + JR[jt]], rhs=w2t[:, k, :], start=(k == 0), stop=False),
                              r=[("actT", s, k), ("w2b", ws)], w=[bt])
                        A("pe", lambda h, b=b, dg=dg, s=s, jt=jt: h.matmul(b[0:JR[jt], :], lhsT=ones_bf[0:1, 0:JR[jt]],
                                                                           rhs=b2r[s][0:1, dg * 512:(dg + 1) * 512], start=False, stop=True),
                          r=["cbf", ("b2r", s)], w=[bt])
                        A("act", lambda h, b=b, jt=jt, dg=dg, yt_=yt_: h.activation(out=yt_[0:JR[jt], jt, dg * 512:(dg + 1) * 512], in_=b[0:JR[jt], :],
                                                                                    func=AF.Copy),
                          r=[bt], w=[("ytok", s, jt)])
                for jt in range(NJT):
                    A("sp", lambda h, jt=jt, yt_=yt_, e=e: h.dma_start(out=yg[e * CAP + jt * 128:e * CAP + jt * 128 + JR[jt], :], in_=yt_[0:JR[jt], jt, :]),
                      r=[("ytok", s, jt)], w=["yg"], dma=True)

            if n_exp > 0:
                moe_prep(0)
            for e in range(n_exp):
                moe_stage1(e)
                if e + 1 < n_exp:
                    moe_prep(e + 1)
                moe_stage2(e)
            barrier()
            P.flush()

        with contextlib.ExitStack() as st:
            def sb(name, shape, dt):
                return st.enter_context(nc.sbuf_tensor(name, list(shape), dt))

            hT = sb("hT_c", [128, KC, NT], F32)
            xn = sb("xn_c", [128, KC, NT], BF16)
            gb = [sb("gb%d" % i, [128, D], F32) for i in range(4)]
            acc = sb("acc", [128, D], F32)
            wple = sb("wple_t", [128, 2, D], BF16)
            ptb = sb("ptb", [128, 2, NT], BF16)
            sqb = [sb("sqb_c%d" % i, [128, NT], BF16) for i in range(2)]
            rs = sb("rs_c", [128, NT], F32)
            rstd = sb("rstd_c", [128, NT], F32)
            gate = [sb("gate%d" % i, [128, NT], F32) for i in range(2)]
            tmp = [sb("tmp%d" % i, [128, NT], F32) for i in range(2)]

            A("pool", lambda h: h.dma_start(out=wple[:], in_=wple_d, max_dma_last_dim=4096), w=["wple"], dma=True)
            for kk in range(4):
                A("dve", lambda h, kk=kk: h.memset(gb[kk][:], 0.0), w=[("gb", kk)])

            def norm_c(gidx, dst_fn, dst_tok):
                b, bt = nb()
                for k in range(KC):
                    sq = sqb[k % 2]
                    A("act", lambda h, k=k, sq=sq: h.activation(out=sq[:], in_=hT[:, k, :], func=AF.Square),
                      r=[("hc", k)], w=[("sqc", k % 2)])
                    A("pe", lambda h, k=k, sq=sq: h.matmul(b[:, :], lhsT=ones_bf, rhs=sq[:], start=(k == 0), stop=(k == KC - 1)),
                      r=[("sqc", k % 2), "cbf"], w=[bt])
                A("act", lambda h: h.activation(out=rs[:], in_=b[:, :], func=AF.Sqrt, scale=1.0 / D, bias=EPS), r=[bt], w=["rsc"])
                A("dve", lambda h: h.reciprocal(out=rstd[:], in_=rs[:]), r=["rsc"], w=["rstdc"])
                for k in range(KC):
                    A("dve", lambda h, k=k: h.scalar_tensor_tensor(
                        out=dst_fn(k), in0=hT[:, k, :], scalar=cf[:, O_GAIN + gidx * 16 + k:O_GAIN + gidx * 16 + k + 1],
                        in1=rstd[:], op0=ALU.mult, op1=ALU.mult),
                      r=[("hc", k), "rstdc", "cf"], w=[(dst_tok, k)])

            for ti in range(n_own if final else 0):
                for half in range(2):
                    A("sp", lambda h, half=half, ti=ti: h.dma_start(out=hT[:, half * 8:(half + 1) * 8, :],
                                                                    in_=hmid[:, half * 8:(half + 1) * 8, ti * NT:(ti + 1) * NT]),
                      r=[("hmid", ti, half)], w=[("hc", k) for k in range(half * 8, half * 8 + 8)], dma=True)
                A("pool", lambda h, ti=ti: h.dma_start(out=ptb[:], in_=pT[:, :, ti * NT:(ti + 1) * NT]), w=["ptb"], dma=True)
                for tb in range(4):
                    blk = ti * 4 + tb
                    for kk in range(4):
                        A("pool", lambda h, kk=kk, blk=blk: h.indirect_dma_start(
                            out=gb[kk][:, :], out_offset=None, in_=yg,
                            in_offset=bass.IndirectOffsetOnAxis(ap=dest_i[:, blk * 4 + kk:blk * 4 + kk + 1], axis=0),
                            bounds_check=P.bc_reg, oob_is_err=False),
                          r=["yg", ("dest_i", blk)], w=[("gb", kk)], dma=True)
                    A("dve", lambda h, blk=blk: h.tensor_scalar(out=acc[:], in0=gb[0][:], scalar1=w4[:, blk * 4:blk * 4 + 1], scalar2=None, op0=ALU.mult),
                      r=[("gb", 0), ("w4", blk)], w=["acc"])
                    for kk in range(1, 4):
                        A("dve", lambda h, kk=kk, blk=blk: h.scalar_tensor_tensor(
                            out=acc[:], in0=gb[kk][:], scalar=w4[:, blk * 4 + kk:blk * 4 + kk + 1], in1=acc[:], op0=ALU.mult, op1=ALU.add),
                          r=[("gb", kk), ("w4", blk), "acc"], w=["acc"])
                    for q4 in range(4):
                        b, bt = nb()
                        for kq in range(4):
                            k = q4 * 4 + kq
                            A("pe", lambda h, k=k, kq=kq, b=b: h.transpose(out=b[:, kq * 128:(kq + 1) * 128], in_=acc[:, k * 128:(k + 1) * 128],
                                                                           identity=ident_f),
                              r=["acc", "cf"], w=[bt])
                        A("dve", lambda h, q4=q4, tb=tb, b=b: h.tensor_tensor(
                            out=hT[:, q4 * 4:(q4 + 1) * 4, tb * 128:(tb + 1) * 128], in0=b[:, :].rearrange("p (a c) -> p a c", c=128),
                            in1=hT[:, q4 * 4:(q4 + 1) * 4, tb * 128:(tb + 1) * 128], op=ALU.add),
                          r=[bt] + [("hc", k) for k in range(q4 * 4, q4 * 4 + 4)], w=[("hc", k) for k in range(q4 * 4, q4 * 4 + 4)])
                norm_c(2, lambda k: xn[:, k, :], "xnc")
                for j in range(KC):
                    wg_, wgt = wstream(wproj[3, j])
                    bg, bgt = nb()
                    for k in range(KC):
                        A("pe", lambda h, k=k, wg_=wg_, bg=bg: h.matmul(bg[:, :], lhsT=wg_[:, k, :], rhs=xn[:, k, :], start=(k == 0), stop=(k == KC - 1)),
                          r=[wgt, ("xnc", k)], w=[bgt])
                    bp, bpt = nb()
                    for k in range(2):
                        A("pe", lambda h, k=k, j=j, bp=bp: h.matmul(bp[:, :], lhsT=wple[:, k, j * 128:(j + 1) * 128], rhs=ptb[:, k, :],
                                                                    start=(k == 0), stop=(k == 1)),
                          r=["wple", "ptb"], w=[bpt])
                    g_ = gate[j % 2]
                    t_ = tmp[j % 2]
                    A("act", lambda h, bg=bg, g_=g_: h.activation(out=g_[:], in_=bg[:, :], func=AF.Sigmoid), r=[bgt], w=[("gate", j % 2)])
                    A("dve", lambda h, bp=bp, g_=g_, t_=t_: h.tensor_tensor(out=t_[:], in0=bp[:, :], in1=g_[:], op=ALU.mult),
                      r=[bpt, ("gate", j % 2)], w=[("tmp", j % 2)])
                    A("dve", lambda h, j=j, t_=t_: h.tensor_tensor(out=hT[:, j, :], in0=hT[:, j, :], in1=t_[:], op=ALU.add),
                      r=[("tmp", j % 2), ("hc", j)], w=[("hc", j)])
                norm_c(3, lambda k: hT[:, k, :], "hc")
                for half in range(2):
                    A("sp", lambda h, half=half, ti=ti: h.dma_start(out=outT[:, half * 8:(half + 1) * 8, ti * NT:(ti + 1) * NT],
                                                                    in_=hT[:, half * 8:(half + 1) * 8, :]),
                      r=[("hc", k) for k in range(half * 8, half * 8 + 8)], w=[("out", ti, half)], dma=True)
            P.flush(final=True)
    return nc


def _chunk_w(w):
    K, M = w.shape
    return np.ascontiguousarray(w.reshape(KC, 128, M // 128, 128).transpose(2, 1, 0, 3))


def _pcol(v):
    return np.ascontiguousarray(v.reshape(-1, 128).T)


def _host_layout(inp):
    f = np.float32
    w_in = inp["w_in"][0]
    b_in = inp["b_in"][0]
    QW, KVW = 2048, 256
    oq, ok, ov = 0, QW, QW + KVW
    ou = QW + 2 * KVW
    og = ou + 2048
    oga = og + 2048
    ogl = oga + 2048
    cols = []
    for c in range(16):
        cols.append(np.arange(oq + c * 128, oq + (c + 1) * 128))
    for kvh in range(4):
        a = np.arange(ok + kvh * 64, ok + (kvh + 1) * 64)
        cols.append(np.concatenate([a, a]))
    for n in range(8):
        cols.append(np.arange(ou + (2 * n) * 128, ou + (2 * n + 1) * 128))
        cols.append(np.arange(ou + (2 * n + 1) * 128, ou + (2 * n + 2) * 128))
        cols.append(np.arange(og + (2 * n) * 128, og + (2 * n + 1) * 128))
        cols.append(np.arange(og + (2 * n + 1) * 128, og + (2 * n + 2) * 128))
    for j in range(16):
        cols.append(np.arange(oga + j * 128, oga + (j + 1) * 128))
        cols.append(np.arange(ogl + j * 128, ogl + (j + 1) * 128))
    cols = np.concatenate(cols)
    assert cols.size == NWIN * 128
    win = _chunk_w(w_in[:, cols])
    bin_ = _pcol(b_in[cols])
    wv = np.ascontiguousarray(w_in[:, ov:ov + 256].reshape(KC, 128, 256).transpose(1, 0, 2))
    bv = np.broadcast_to(b_in[ov:ov + 256][None, :], (128, 256))
    wproj = np.stack([_chunk_w(inp["w_attn_proj"][0]), _chunk_w(inp["w_lru_proj"][0]),
                      _chunk_w(inp["w_out"][0]), _chunk_w(inp["w_ple_gate"][0])])
    wa = inp["w_rg_a"][0].reshape(8, 2, 128, 256).transpose(0, 2, 1, 3)
    wx = inp["w_rg_x"][0].reshape(8, 2, 128, 256).transpose(0, 2, 1, 3)
    wrg = np.ascontiguousarray(np.stack([wa, wx], axis=2))
    brg = np.concatenate([_pcol(inp["b_rg_a"][0].reshape(-1)), _pcol(inp["b_rg_x"][0].reshape(-1))], axis=1)
    convw = np.concatenate([_pcol(inp["conv_w"][0][t]) for t in range(4)], axis=1)
    convb = _pcol(inp["conv_b"][0])
    lam = _pcol(inp["lru_lambda"][0])
    gains = np.concatenate([_pcol(inp["norm_mix_g"][0]), _pcol(inp["norm_ffn_g"][0]),
                            _pcol(inp["norm_ple_g"][0]), _pcol(inp["norm_final_g"])], axis=1)
    sink = _pcol(np.repeat(inp["attn_sinks"][0], 64))
    wrouter = np.ascontiguousarray(inp["w_router"][0].reshape(KC, 128, NE).transpose(1, 0, 2))
    brout = np.broadcast_to(inp["b_router"][0][None, :], (128, NE))
    wple = np.ascontiguousarray(inp["w_ple"][0].reshape(2, 128, D).transpose(1, 0, 2))
    w1 = inp["w_mlp1"][0]
    w1l = np.ascontiguousarray(w1.reshape(NE, KC, 128, 32, 128).transpose(0, 3, 2, 1, 4))
    b1 = inp["b_mlp1"][0].reshape(NE, 32, 128).transpose(2, 0, 1).reshape(128, NE * 32)
    w2 = inp["w_mlp2"][0]
    w2l = np.ascontiguousarray(w2.reshape(NE, KC, 128, 4, 512).transpose(0, 3, 2, 1, 4))
    b2 = np.ascontiguousarray(inp["b_mlp2"][0])
    ecap = np.broadcast_to((np.arange(NE, dtype=f) * CAP - BIG)[None, :], (128, NE))
    cf = np.zeros((128, NCF), f)
    cf[:, O_IDF:O_IDF + 128] = np.eye(128, dtype=f)
    cf[:, O_ECAP:O_ECAP + NE] = ecap
    cf[:, O_BROUT:O_BROUT + NE] = brout
    cf[:, O_BV:O_BV + 256] = bv
    cf[:, O_BIN:O_BIN + NWIN] = bin_
    cf[:, O_BRG:O_BRG + 32] = brg
    cf[:, O_CONVW:O_CONVW + 64] = convw
    cf[:, O_CONVB:O_CONVB + 16] = convb
    cf[:, O_LAM:O_LAM + 16] = lam
    cf[:, O_GAIN:O_GAIN + 64] = gains
    cf[:, O_SINK:O_SINK + 16] = sink
    cf[:, O_B1:O_B1 + 1024] = b1
    NEG = -30000.0
    jj = np.arange(128)[:, None]
    ii = np.arange(128)[None, :]
    mbprev = np.where(jj > ii, 0.0, NEG).astype(f)
    mbcur = np.where(jj <= ii, 0.0, NEG).astype(f)
    cbf = np.zeros((128, NCB), f)
    cbf[:, B_ID:B_ID + 128] = np.eye(128, dtype=f)
    cbf[:, B_LT:B_LT + 128] = (jj < ii).astype(f)
    cbf[:, B_ONES:B_ONES + 128] = 1.0
    cbf[:, B_MB4:B_MB4 + 512] = np.concatenate([mbprev, mbcur, mbprev, mbcur], axis=1)
    cbf[:, B_OLO:B_OLO + 64] = 1.0
    cbf[:, B_OHI + 64:B_OHI + 128] = 1.0
    shared = dict(cbf=cbf, cf=cf, win=win, wv=wv, wproj=wproj, wrg=wrg, wrouter=wrouter, wple=wple,
                  w1=w1l, w2=w2l, b2=b2)
    x = inp["x"]
    p = inp["p"][0]
    in_maps = []
    for core in range(NCORES):
        b, half = core // 2, core % 2
        xt = x[b].T.reshape(KC, 128, 2 * TOWN).transpose(1, 0, 2)
        if half == 0:
            xT = np.concatenate([np.zeros((128, KC, TOWN), f), xt[:, :, :TOWN]], axis=2)
        else:
            xT = xt
        pt = p[b, half * TOWN:(half + 1) * TOWN].T.reshape(2, 128, TOWN).transpose(1, 0, 2)
        ccv = np.zeros((128, 516), f)
        mb0 = np.full((128, 128), NEG, f) if half == 0 else mbprev
        ccv[:, 0:512] = np.concatenate([mb0, mbcur, mb0, mbcur], axis=1)
        ccv[:, 512] = float(half)
        ccv[:, 513] = float(half)
        ccv[:, 514] = float(1 - half)
        m = dict(shared)
        m["xT"] = np.ascontiguousarray(xT, dtype=f)
        m["pT"] = np.ascontiguousarray(pt, dtype=f)
        m["cc"] = ccv
        in_maps.append(m)
    return in_maps


_NC_CACHE = {}


def kernel(**inputs):
    inp = {k: np.asarray(v) for k, v in inputs.items()}
    in_maps = _host_layout(inp)
    if "nc" not in _NC_CACHE:
        _NC_CACHE["nc"] = build_nc()
    nc = _NC_CACHE["nc"]
    res = run_bass_kernel_spmd(nc, in_maps, core_ids=list(range(NCORES)))
    out = np.empty((4, 4096, D), np.float32)
    for core in range(NCORES):
        b, half = core // 2, core % 2
        o = res.results[core]["outT"]
        out[b, half * TOWN:(half + 1) * TOWN, :] = o.transpose(2, 1, 0).reshape(TOWN, D)
    return out
```
